# Optimizing a Trainium2 kernel written in Bass

```python
import jax, jax.numpy as jnp
from jax import lax
import numpy as np

D_MODEL = 4096
BATCH = 1
SEQ = 16384
DEPTH = 1

GRID_W = 64
CTX_LEN = 256
GDN_HEADS = 16
GDN_HEAD_DIM = 128
GDN_WIDTH = GDN_HEADS * GDN_HEAD_DIM
GDN_CHUNK = 64
QKV_CONV = 3
CONV_WIDTH = D_MODEL // 2
SHORT_CONV = 3
N_GROUPS = 8
EXPERTS_PER_GROUP = 8
N_EXPERTS = N_GROUPS * EXPERTS_PER_GROUP
TOP_K_IN_GROUP = 2
EXPERT_HIDDEN = 768
ROUTE_BLOCK = 128
N_MOD = 6
IN_COLS = 4 * GDN_WIDTH + 4 * GDN_HEADS + 3 * CONV_WIDTH + 2 * D_MODEL
DEEPNORM_ALPHA = (2 * DEPTH) ** 0.25
DEEPNORM_BETA = (8 * DEPTH) ** -0.25
LN_EPS = 1e-6
RMS_EPS = 1e-6

kernel_name = "hybrid_gdn_shortconv_hmoe_diffusion_block"


def layer_norm(x, gain=None, bias=None):
    xf = x.astype(jnp.float32)
    mu = jnp.mean(xf, axis=-1, keepdims=True)
    var = jnp.mean(jnp.square(xf - mu), axis=-1, keepdims=True)
    y = (xf - mu) * lax.rsqrt(var + LN_EPS)
    if gain is not None:
        y = y * gain.astype(jnp.float32) + bias.astype(jnp.float32)
    return y.astype(x.dtype)


def modulate(h, shift, scale):
    return h * (1 + scale) + shift


def l2_normalize(t):
    return t * lax.rsqrt(jnp.sum(t * t, axis=-1, keepdims=True) + 1e-6)


def dwconv_centred(x, w):
    k = w.shape[0]
    pad = k // 2
    n = x.shape[-2]
    xp = jnp.pad(x, [(0, 0)] * (x.ndim - 2) + [(pad, pad), (0, 0)])
    return sum(w[i] * xp[..., i:i + n, :] for i in range(k))


def conv_latent(x, w):
    b, n, ch = x.shape
    rows = n // GRID_W
    return dwconv_centred(x.reshape(b, rows, GRID_W, ch), w).reshape(b, n, ch)


def split_projection(p):
    sizes = (3 * GDN_WIDTH, GDN_WIDTH, 2 * GDN_HEADS, 2 * GDN_HEADS, 3 * CONV_WIDTH, 2 * D_MODEL)
    return jnp.split(p, np.cumsum(sizes)[:-1].tolist(), axis=-1)


def gdn_inputs(qkv, beta_raw, a_raw, conv_fn, conv_qkv, a_log, dt_bias):
    b, n, _ = qkv.shape
    qkv = jax.nn.silu(conv_fn(qkv, conv_qkv)).astype(jnp.float32)
    q, k, v = (t.reshape(b, n, GDN_HEADS, GDN_HEAD_DIM) for t in jnp.split(qkv, 3, axis=-1))
    q = l2_normalize(q) * GDN_HEAD_DIM ** -0.5
    k = l2_normalize(k)
    beta = jax.nn.sigmoid(beta_raw.astype(jnp.float32)).reshape(b, n, 2, GDN_HEADS)
    a = a_raw.astype(jnp.float32).reshape(b, n, 2, GDN_HEADS)
    g = -jnp.exp(a_log.astype(jnp.float32)) * jax.nn.softplus(a + dt_bias.astype(jnp.float32))
    return q, k, v, g, beta


def gdn_chunked(q, k, v, g, beta, s0):
    b, n, h, _ = q.shape
    nc = n // GDN_CHUNK

    def chunks(t):
        return jnp.moveaxis(t.reshape((b, nc, GDN_CHUNK, h) + t.shape[3:]), 3, 1)

    q, k, v, g, beta = (chunks(t) for t in (q, k, v, g, beta))
    g = jnp.cumsum(g, axis=-1)
    incl = jnp.tril(jnp.ones((GDN_CHUNK, GDN_CHUNK), bool))
    strict = jnp.tril(jnp.ones((GDN_CHUNK, GDN_CHUNK), bool), -1)
    decay = jnp.exp(jnp.where(incl, g[..., :, None] - g[..., None, :], -jnp.inf))
    kb = k * beta[..., None]
    lmat = jnp.where(strict, jnp.einsum('bhnid,bhnjd->bhnij', kb, k) * decay, 0.0)
    amat = lmat + jnp.eye(GDN_CHUNK, dtype=jnp.float32)
    u = lax.linalg.triangular_solve(amat, v * beta[..., None], left_side=True, lower=True, unit_diagonal=True)
    w = lax.linalg.triangular_solve(amat, kb * jnp.exp(g)[..., None], left_side=True, lower=True, unit_diagonal=True)
    attn = jnp.einsum('bhnid,bhnjd->bhnij', q, k) * decay
    qg = q * jnp.exp(g)[..., None]
    kd = k * jnp.exp(g[..., -1:] - g)[..., None]
    g_last = jnp.exp(g[..., -1])
    xs = tuple(jnp.moveaxis(t, 2, 0) for t in (u, w, attn, qg, kd, g_last))

    def step(s, xc):
        u_c, w_c, attn_c, qg_c, kd_c, gl_c = xc
        v_new = u_c - jnp.einsum('bhck,bhkv->bhcv', w_c, s)
        o_c = jnp.einsum('bhck,bhkv->bhcv', qg_c, s) + jnp.einsum('bhij,bhjv->bhiv', attn_c, v_new)
        s = s * gl_c[..., None, None] + jnp.einsum('bhck,bhcv->bhkv', kd_c, v_new)
        return s, o_c

    s_final, o = lax.scan(step, s0, xs)
    o = jnp.transpose(o, (1, 0, 3, 2, 4)).reshape(b, n, h, v.shape[-1])
    return o, s_final


def gdn_bidirectional(q, k, v, g, beta, s0):
    b = q.shape[0]
    both = lambda t: jnp.concatenate([t, jnp.flip(t, axis=1)], axis=0)
    per_dir = lambda t: jnp.concatenate([t[:, :, 0], jnp.flip(t[:, :, 1], axis=1)], axis=0)
    o, s = gdn_chunked(both(q), both(k), both(v), per_dir(g), per_dir(beta), s0)
    return o[:b] + jnp.flip(o[b:], axis=1), s


def gated_rms_norm(o, z, w):
    zf = z.astype(jnp.float32).reshape(o.shape)
    y = o * lax.rsqrt(jnp.mean(o * o, axis=-1, keepdims=True) + RMS_EPS) * w.astype(jnp.float32) * jax.nn.silu(zf)
    return y.reshape(o.shape[0], o.shape[1], GDN_WIDTH).astype(z.dtype)


def short_conv_mixer(xbc, conv_fn, conv_b):
    xb, bg, cg = jnp.split(xbc, 3, axis=-1)
    return bg * conv_fn(cg * xb, conv_b)


def merge_branches(y_gdn, y_conv, gates, w_branch_a, w_branch_b, w_out):
    gate_a, gate_b = jnp.split(gates, 2, axis=-1)
    m = jax.nn.sigmoid(gate_a) * (y_gdn @ w_branch_a) + jax.nn.sigmoid(gate_b) * (y_conv @ w_branch_b)
    return m @ w_out


def hierarchical_moe(h, w_router_group, b_router_group, w_router_expert, b_router_expert,
                     w_exp_gate, w_exp_up, w_exp_down):
    n, d = h.shape
    hf = h.astype(jnp.float32)
    p_group = jax.nn.softmax(hf @ w_router_group.astype(jnp.float32) + b_router_group.astype(jnp.float32), axis=-1)
    group = jnp.argmax(p_group, axis=-1)
    gate_group = jnp.take_along_axis(p_group, group[:, None], axis=-1)
    logits_e = (hf @ w_router_expert.astype(jnp.float32) + b_router_expert.astype(jnp.float32)).reshape(n, N_GROUPS, EXPERTS_PER_GROUP)
    logits_e = jnp.take_along_axis(logits_e, group[:, None, None], axis=1)[:, 0]
    top_p, top_i = lax.top_k(jax.nn.softmax(logits_e, axis=-1), TOP_K_IN_GROUP)
    weights = (gate_group * top_p / jnp.sum(top_p, axis=-1, keepdims=True)).reshape(-1)
    expert_id = (group[:, None] * EXPERTS_PER_GROUP + top_i).reshape(-1)
    n_assign = n * TOP_K_IN_GROUP
    token_id = jnp.repeat(jnp.arange(n), TOP_K_IN_GROUP)
    order = jnp.argsort(expert_id)
    e_s, tok_s, w_s = expert_id[order], token_id[order], weights[order]
    counts = jnp.bincount(expert_id, length=N_EXPERTS)
    padded = (counts + ROUTE_BLOCK - 1) // ROUTE_BLOCK * ROUTE_BLOCK
    pad_end = jnp.cumsum(padded)
    pad_start = pad_end - padded
    start = jnp.cumsum(counts) - counts
    dest = pad_start[e_s] + jnp.arange(n_assign) - start[e_s]
    n_blocks = -(-n_assign // ROUTE_BLOCK) + N_EXPERTS
    buf = jnp.zeros((n_blocks * ROUTE_BLOCK, d), h.dtype).at[dest].set(h[tok_s])
    block_e = jnp.minimum(jnp.searchsorted(pad_end, jnp.arange(n_blocks) * ROUTE_BLOCK, side='right'), N_EXPERTS - 1)

    def expert_block(args):
        xb, e = args
        return (jax.nn.silu(xb @ w_exp_gate[e]) * (xb @ w_exp_up[e])) @ w_exp_down[e]

    y = lax.map(expert_block, (buf.reshape(n_blocks, ROUTE_BLOCK, d), block_e)).reshape(-1, d)
    y = y[dest] * w_s[:, None].astype(h.dtype)
    return jax.ops.segment_sum(y, tok_s, num_segments=n)


def trunk_layer(x, ctx, c, c_ctx, w_ada, b_ada, w_in, conv_qkv, a_log, dt_bias, gdn_norm_w, conv_b,
                w_branch_a, w_branch_b, w_out, ln1_g, ln1_b, w_router_group, b_router_group,
                w_router_expert, b_router_expert, w_exp_gate, w_exp_up, w_exp_down, ln2_g, ln2_b, need_ctx):
    b, n, d = x.shape
    mod_lat = jnp.split((jax.nn.silu(c) @ w_ada + b_ada)[:, None, :], N_MOD, axis=-1)
    mod_ctx = jnp.split(jax.nn.silu(c_ctx) @ w_ada + b_ada, N_MOD, axis=-1)

    h_lat = modulate(layer_norm(x), mod_lat[0], mod_lat[1])
    h_ctx = modulate(layer_norm(ctx), mod_ctx[0], mod_ctx[1])
    qkv_l, z_l, beta_l, a_l, xbc_l, gates_l = split_projection(h_lat @ w_in)
    qkv_c, z_c, beta_c, a_c, xbc_c, gates_c = split_projection(h_ctx @ w_in)
    q_c, k_c, v_c, g_c, bt_c = gdn_inputs(qkv_c, beta_c, a_c, dwconv_centred, conv_qkv, a_log, dt_bias)
    q_l, k_l, v_l, g_l, bt_l = gdn_inputs(qkv_l, beta_l, a_l, conv_latent, conv_qkv, a_log, dt_bias)
    s0 = jnp.zeros((2 * b, GDN_HEADS, GDN_HEAD_DIM, GDN_HEAD_DIM), jnp.float32)
    o_c, s_c = gdn_bidirectional(q_c, k_c, v_c, g_c, bt_c, s0)
    o_l, _ = gdn_bidirectional(q_l, k_l, v_l, g_l, bt_l, s_c)
    mix_l = merge_branches(gated_rms_norm(o_l, z_l, gdn_norm_w), short_conv_mixer(xbc_l, conv_latent, conv_b),
                           gates_l, w_branch_a, w_branch_b, w_out)
    x = layer_norm(DEEPNORM_ALPHA * x + mod_lat[2] * mix_l, ln1_g, ln1_b)
    if need_ctx:
        mix_c = merge_branches(gated_rms_norm(o_c, z_c, gdn_norm_w), short_conv_mixer(xbc_c, dwconv_centred, conv_b),
                               gates_c, w_branch_a, w_branch_b, w_out)
        ctx = layer_norm(DEEPNORM_ALPHA * ctx + mod_ctx[2] * mix_c, ln1_g, ln1_b)

    tokens = modulate(layer_norm(x), mod_lat[3], mod_lat[4]).reshape(-1, d)
    if need_ctx:
        tokens = jnp.concatenate([tokens, modulate(layer_norm(ctx), mod_ctx[3], mod_ctx[4]).reshape(-1, d)], axis=0)
    f = hierarchical_moe(tokens, w_router_group, b_router_group, w_router_expert, b_router_expert,
                         w_exp_gate, w_exp_up, w_exp_down)
    x = layer_norm(DEEPNORM_ALPHA * x + mod_lat[5] * f[:b * n].reshape(b, n, d), ln2_g, ln2_b)
    if need_ctx:
        ctx = layer_norm(DEEPNORM_ALPHA * ctx + mod_ctx[5] * f[b * n:].reshape(ctx.shape), ln2_g, ln2_b)
    return x, ctx


def setup_inputs(seed: int = 0) -> dict:
    key = jax.random.key(seed)
    ks = jax.random.split(key, 26)
    nrm = lambda k, shape, s: jax.random.normal(k, shape, jnp.float32) * s
    dt = jnp.exp(jax.random.uniform(ks[9], (DEPTH, 2, GDN_HEADS), jnp.float32, np.log(1e-3), np.log(1e-1)))
    return {
        "x": nrm(ks[0], (BATCH, SEQ, D_MODEL), 1.0),
        "c": nrm(ks[1], (BATCH, D_MODEL), 1.0),
        "ctx": nrm(ks[2], (BATCH, CTX_LEN, D_MODEL), 1.0),
        "c_ctx": nrm(ks[3], (D_MODEL,), 1.0),
        "w_ada": nrm(ks[4], (DEPTH, D_MODEL, N_MOD * D_MODEL), 0.5 * D_MODEL ** -0.5),
        "b_ada": nrm(ks[5], (DEPTH, N_MOD * D_MODEL), 0.02),
        "w_in": nrm(ks[6], (DEPTH, D_MODEL, IN_COLS), D_MODEL ** -0.5),
        "conv_qkv": nrm(ks[7], (DEPTH, QKV_CONV, 3 * GDN_WIDTH), QKV_CONV ** -0.5),
        "a_log": jnp.log(jax.random.uniform(ks[8], (DEPTH, 2, GDN_HEADS), jnp.float32, 1.0, 16.0)),
        "dt_bias": dt + jnp.log(-jnp.expm1(-dt)),
        "gdn_norm_w": 1.0 + nrm(ks[10], (DEPTH, GDN_HEAD_DIM), 0.02),
        "conv_b": nrm(ks[11], (DEPTH, SHORT_CONV, CONV_WIDTH), SHORT_CONV ** -0.5),
        "w_branch_a": nrm(ks[12], (DEPTH, GDN_WIDTH, D_MODEL), GDN_WIDTH ** -0.5),
        "w_branch_b": nrm(ks[13], (DEPTH, CONV_WIDTH, D_MODEL), CONV_WIDTH ** -0.5),
        "w_out": nrm(ks[14], (DEPTH, D_MODEL, D_MODEL), DEEPNORM_BETA * D_MODEL ** -0.5),
        "ln1_g": 1.0 + nrm(ks[15], (DEPTH, D_MODEL), 0.02),
        "ln1_b": nrm(ks[16], (DEPTH, D_MODEL), 0.02),
        "w_router_group": nrm(ks[17], (DEPTH, D_MODEL, N_GROUPS), D_MODEL ** -0.5),
        "b_router_group": nrm(ks[18], (DEPTH, N_GROUPS), 0.01),
        "w_router_expert": nrm(ks[19], (DEPTH, D_MODEL, N_EXPERTS), D_MODEL ** -0.5),
        "b_router_expert": nrm(ks[20], (DEPTH, N_EXPERTS), 0.01),
        "w_exp_gate": nrm(ks[21], (DEPTH, N_EXPERTS, D_MODEL, EXPERT_HIDDEN), D_MODEL ** -0.5),
        "w_exp_up": nrm(ks[22], (DEPTH, N_EXPERTS, D_MODEL, EXPERT_HIDDEN), D_MODEL ** -0.5),
        "w_exp_down": nrm(ks[23], (DEPTH, N_EXPERTS, EXPERT_HIDDEN, D_MODEL), DEEPNORM_BETA * EXPERT_HIDDEN ** -0.5),
        "ln2_g": 1.0 + nrm(ks[24], (DEPTH, D_MODEL), 0.02),
        "ln2_b": nrm(ks[25], (DEPTH, D_MODEL), 0.02),
    }


def reference(x, c, ctx, c_ctx, w_ada, b_ada, w_in, conv_qkv, a_log, dt_bias, gdn_norm_w, conv_b,
              w_branch_a, w_branch_b, w_out, ln1_g, ln1_b, w_router_group, b_router_group,
              w_router_expert, b_router_expert, w_exp_gate, w_exp_up, w_exp_down, ln2_g, ln2_b):
    for layer in range(DEPTH):
        x, ctx = trunk_layer(
            x, ctx, c, c_ctx, w_ada[layer], b_ada[layer], w_in[layer], conv_qkv[layer], a_log[layer],
            dt_bias[layer], gdn_norm_w[layer], conv_b[layer], w_branch_a[layer], w_branch_b[layer],
            w_out[layer], ln1_g[layer], ln1_b[layer], w_router_group[layer], b_router_group[layer],
            w_router_expert[layer], b_router_expert[layer], w_exp_gate[layer], w_exp_up[layer],
            w_exp_down[layer], ln2_g[layer], ln2_b[layer], need_ctx=layer < DEPTH - 1)
    return x
```

```python
import numpy as np
from contextlib import ExitStack
import concourse.bass as bass
import concourse.mybir as mybir
from concourse.bass_utils import run_bass_kernel_spmd

F32 = mybir.dt.float32
BF16 = mybir.dt.bfloat16
I32 = mybir.dt.int32
U8 = mybir.dt.uint8
AF = mybir.ActivationFunctionType
ALU = mybir.AluOpType
AX = mybir.AxisListType

NCORE = 8
D = 4096
KC = 32
NTOK = 16384
NCTX = 256
TOWN = NTOK // NCORE
GT = 256
RW = 644
NEG = -30000.0
LN_EPS = 1e-6
ALPHA = 2.0 ** 0.25


class Buf:
    __slots__ = ("name", "last_write", "reads", "excl")

    def __init__(self, name="", excl=False):
        self.name = name
        self.last_write = None
        self.reads = []
        self.excl = excl


class Chan:
    def __init__(self, sem):
        self.sem = sem
        self.count = 0


class Prog:
    ENGS = ("pe", "act", "dve", "pool", "sp")

    def __init__(self, nc, stack, same_engine_sync=True):
        self.nc = nc
        self.stack = stack
        self.same_engine_sync = same_engine_sync
        self.sem = {e: stack.enter_context(nc.semaphore("s_" + e)) for e in self.ENGS}
        self.count = {e: 0 for e in self.ENGS}
        self.ops = {e: [] for e in self.ENGS}
        self.waited = {e: {} for e in self.ENGS}
        self.chans = []
        self.n_waits = 0
        self.n_ops = 0

    def chan(self, name=None):
        s = self.stack.enter_context(self.nc.semaphore(name or ("c%d" % len(self.chans))))
        c = Chan(s)
        self.chans.append(c)
        return c

    def _collect(self, reads, writes, skip_chan=None):
        out = {}

        def add(tok):
            k, v = tok
            if skip_chan is not None and k is skip_chan:
                return
            if k not in out or out[k] < v:
                out[k] = v
        for b in reads:
            if b.last_write is not None:
                add(b.last_write)
        for b in writes:
            if b.last_write is not None:
                add(b.last_write)
            for t in b.reads:
                add(t)
        return out

    def _waits(self, eng, deps):
        ws = []
        for k, v in deps.items():
            if isinstance(k, str):
                if k == eng and (eng == "pe" or not self.same_engine_sync):
                    continue
                sem = self.sem[k]
            else:
                sem = k.sem
            w = self.waited[eng]
            if w.get(id(sem), -1) >= v:
                continue
            w[id(sem)] = v
            ws.append((sem, v))
        self.n_waits += len(ws)
        return ws

    def _record(self, tok, reads, writes):
        for b in writes:
            b.last_write = tok
            b.reads = []
        for b in reads:
            if b not in writes:
                b.reads.append(tok)

    @staticmethod
    def _split(reads, writes):
        ex = [b for b in reads if b.excl and b not in writes]
        if ex:
            return [b for b in reads if not b.excl], list(writes) + ex
        return reads, writes

    def op(self, eng, fn, reads=(), writes=()):
        reads, writes = self._split(reads, writes)
        deps = self._collect(reads, writes)
        ws = self._waits(eng, deps)
        self.count[eng] += 1
        self.n_ops += 1
        self.ops[eng].append((ws, fn, self.sem[eng], 1))
        tok = (eng, self.count[eng])
        self._record(tok, reads, writes)
        return tok

    def dma(self, eng, fn, chan, reads=(), writes=(), inc=16):
        reads, writes = self._split(reads, writes)
        deps = self._collect(reads, writes, skip_chan=chan)
        ws = self._waits(eng, deps)
        chan.count += inc
        self.n_ops += 1
        self.ops[eng].append((ws, fn, chan.sem, inc))
        tok = (chan, chan.count)
        self._record(tok, reads, writes)
        return tok

    def barrier(self):
        deps = {e: self.count[e] for e in self.ENGS if self.count[e] > 0}
        for c in self.chans:
            if c.count > 0:
                deps[c] = c.count
        for e in self.ENGS:
            ws = self._waits(e, dict(deps))
            if ws:
                self.ops[e].append((ws, None, None, 0))

    def emit(self):
        nc = self.nc
        handles = {"pe": "tensor", "act": "scalar", "dve": "vector", "pool": "gpsimd", "sp": "sync"}
        with nc.Block() as block:
            for e in self.ENGS:
                ops = self.ops[e]
                if not ops:
                    continue

                def body(engine, ops=ops):
                    for (ws, fn, sem, inc) in ops:
                        for (s, v) in ws:
                            engine.wait_ge(s, v)
                        if fn is not None:
                            ins = fn(engine)
                            ins.then_inc(sem, inc)
                getattr(block, handles[e])(body)


_DTB = {F32: 4, BF16: 2, I32: 4}


class Arena:
    def __init__(self, ap, size):
        self.ap = ap
        self.size = size
        self.off = 0

    def reset(self, off=0):
        self.off = off

    def alloc(self, shape, dt):
        assert shape[0] == 128
        n = int(np.prod(shape[1:])) * _DTB[dt]
        n_al = (n + 63) // 64 * 64
        assert self.off + n_al <= self.size, ("arena overflow", self.off, n_al, self.size)
        v = self.ap[:, self.off:self.off + n].bitcast(dt)
        self.off += n_al
        if len(shape) == 3:
            v = v.rearrange("p (a b) -> p a b", b=shape[2])
        elif len(shape) == 4:
            v = v.rearrange("p (a b c) -> p a b c", b=shape[2], c=shape[3])
        return v


def build_program(n_lat_groups=NTOK // GT, dbg=False, phase_b=True, stop=None, moe_blocks=None):
    nc = bass.Bass("TRN2", target_bir_lowering=False)

    def din(name, shape, dt=F32):
        return nc.dram_tensor(name, list(shape), dt, kind="ExternalInput").ap()

    def dout(name, shape, dt=F32):
        return nc.dram_tensor(name, list(shape), dt, kind="ExternalOutput").ap()

    def dscr(name, shape, dt=F32):
        return nc.dram_tensor(name, list(shape), dt).ap()

    x_own = din("x_own", [TOWN, D])
    xidx_in = din("xidx", [128, 16], I32)
    x_part = dscr("x_part", [NTOK, D])
    x_full = dscr("x_all", [NTOK, D])
    ctx_in = din("ctx", [NCTX, D])
    c2_in = din("c2", [128, KC, 2])
    w_ada_s = din("w_ada_s", [D, 3072])
    b_ada_fm = din("b_ada_fm", [128, 24])
    modidx_in = din("modidx", [128, 1], I32)
    w_qkvz = din("w_qkvz", [D, 1024])
    w_ab = din("w_ab", [D, 8])
    convq_in = din("convq", [128, 6, 3])
    alog_in = din("alog_bc", [128, 4])
    dtb_in = din("dtb_bc", [128, 4])
    normw_in = din("normw", [128, 1])
    yidx_in = din("yidx", [128, 16], I32)
    ident_in = din("ident", [128, 128])
    ones_in = din("ones", [128, 128])
    tri4_in = din("tri4", [128, 4, 128])
    nbu_in = din("nbu", [128, 4, 128])
    nbl_in = din("nbl", [128, 4, 128])
    ident4_in = din("ident4", [128, 4, 128])
    lvlmask_in = din("lvlmask", [128, 7, 128])

    if phase_b:
        w_in_b_own = din("w_in_b_own", [512, 14336])
        widx_in = din("widx", [128, 4], I32)
        w_in_b_part = dscr("w_in_b_part", [D, 14336])
        w_in_b = dscr("w_in_b_all", [D, 14336])
        convb_in = din("convb", [128, 16, 3])
        w_ba = din("w_ba", [2048, D])
        w_bb = din("w_bb", [2048, D])
        w_o = din("w_o", [D, D])
        ln1g_in = din("ln1g_bc", [128, D])
        ln1b_in = din("ln1b_bc", [128, D])
        ln2g_in = din("ln2g_bc", [128, D])
        ln2b_in = din("ln2b_bc", [128, D])
        w_router = din("w_router", [D, 72])
        brouter_in = din("brouter_bc", [128, 72])
        if stop is None:
            eidx_in = din("eidx", [128, 8], I32)
            w_eg_own = din("w_eg_own", [4, 1024, 6144])
            w_eu_own = din("w_eu_own", [4, 1024, 6144])
            w_ed_own = din("w_ed_own", [6, 1024, 4096])
            w_eg_part = [dscr("w_eg_part%d" % q, [8192, 6144]) for q in range(4)]
            w_eu_part = [dscr("w_eu_part%d" % q, [8192, 6144]) for q in range(4)]
            w_ed_part = [dscr("w_ed_part%d" % q, [8192, 4096]) for q in range(6)]
            w_eg = [dscr("w_eg_all%d" % q, [8192, 6144]) for q in range(4)]
            w_eu = [dscr("w_eu_all%d" % q, [8192, 6144]) for q in range(4)]
            w_ed = [dscr("w_ed_all%d" % q, [8192, 4096]) for q in range(6)]
        thr_in = din("thr", [128, 96, 64])
        thr2_in = din("thr2", [128, 64, 32])
        iotap_in = din("iota_p", [128, 1])
        ustrict_in = din("ustrict", [128, 128])
    out_d = dout("out", [TOWN, D])
    n_chunks = 2 + n_lat_groups * 2
    n_lat_tok = n_lat_groups * GT

    mod_part = dscr("mod_part", [NCORE * 128, 48])
    mod_all = dscr("mod_all", [NCORE * 128, 48])
    rec_d = dscr("rec_d", [n_chunks * 4, 128, RW], BF16)
    z_scr = dscr("z_scr", [2, 128, NTOK], BF16)
    ybuf = dscr("ybuf", [NCORE * 2048, TOWN], BF16)
    yown = dscr("yown", [2048, TOWN], BF16)
    mod_nat = dscr("mod_nat", [6, D])
    x1_scr = dscr("x1_scr", [TOWN, D])
    n2_scr = dscr("n2_scr", [TOWN, D], BF16)
    moe_buf = dscr("moe_buf", [96 * 128, D], BF16)
    moe_y = dscr("moe_y", [96 * 128, D])

    dbg_t = {}
    if dbg:
        dbg_t["mod"] = dout("dbg_mod", [128, 384])
        dbg_t["qkv"] = dout("dbg_qkv", [128, 6, GT])
        dbg_t["qkn"] = dout("dbg_qkn", [128, 6, GT], BF16)
        dbg_t["gb"] = dout("dbg_gb", [128, 2, 8])
        dbg_t["rec"] = dout("dbg_rec", [4, 128, RW], BF16)
        dbg_t["S"] = dout("dbg_S", [128, 4, 128])
        dbg_t["Sc"] = dout("dbg_Sc", [128, 4, 128])
        dbg_t["o"] = dout("dbg_o", [128, 2, 1024])
        dbg_t["yown"] = dout("dbg_yown", [2048, TOWN], BF16)
        dbg_t["aa"] = dout("dbg_aa", [128, 16, 128])
        dbg_t["w12"] = dout("dbg_w12", [128, 16, 2])
        dbg_t["destf"] = dout("dbg_destf", [128, 16, 2])
        dbg_t["bef"] = dout("dbg_bef", [128, 96])
        dbg_t["x1"] = dout("dbg_x1", [TOWN, D])

    with ExitStack() as st:
        P = Prog(nc, st)
        ARENA_BYTES = 204800
        arena_t = st.enter_context(nc.sbuf_tensor("arena", [128, ARENA_BYTES], U8))
        A = Arena(arena_t, ARENA_BYTES)
        banks = [st.enter_context(nc.psum_tensor("bank%d" % i, [128, 512], F32)) for i in range(8)]
        BK = [Buf("bank%d" % i, excl=True) for i in range(8)]

        def bkbf(i):
            return banks[i][:, :].bitcast(BF16)

        def act(out, in_, func, reads, writes, **kw):
            P.op("act", lambda e: e.activation(out=out, in_=in_, func=func, **kw), reads, writes)

        def tt(eng, out, in0, in1, op, reads, writes):
            P.op(eng, lambda e: e.tensor_tensor(out=out, in0=in0, in1=in1, op=op), reads, writes)

        def ts(eng, out, in0, s1, s2, op0, op1, reads, writes):
            if s2 is None:
                P.op(eng, lambda e: e.tensor_scalar(out=out, in0=in0, scalar1=s1, scalar2=None, op0=op0), reads, writes)
            else:
                P.op(eng, lambda e: e.tensor_scalar(out=out, in0=in0, scalar1=s1, scalar2=s2, op0=op0, op1=op1), reads, writes)

        def stt(eng, out, in0, scalar, in1, op0, op1, reads, writes):
            P.op(eng, lambda e: e.scalar_tensor_tensor(out=out, in0=in0, scalar=scalar, in1=in1, op0=op0, op1=op1), reads, writes)

        def cp(eng, out, in_, reads, writes):
            if eng == "act":
                act(out, in_, AF.Identity, reads, writes)
            else:
                P.op(eng, lambda e: e.tensor_copy(out=out, in_=in_), reads, writes)

        def dma(eng, out, in_, chan, reads, writes):
            P.dma(eng, lambda e: e.dma_start(out=out, in_=in_), chan, reads, writes)

        ident_f = A.alloc([128, 128], F32)
        ident_b = A.alloc([128, 128], BF16)
        ones_f = A.alloc([128, 128], F32)
        ones_b = A.alloc([128, 128], BF16)
        modflat = A.alloc([128, 384], F32)
        mod1p = A.alloc([128, 384], F32)
        CONST = Buf("const")
        MOD = Buf("mod")
        ch_c = P.chan("ch_const")
        dma("sp", ident_f, ident_in, ch_c, [], [CONST])
        dma("sp", ones_f, ones_in, ch_c, [], [CONST])
        cp("dve", ident_b, ident_f, [CONST], [CONST])
        cp("dve", ones_b, ones_f, [CONST], [CONST])
        persist_off = A.off

        def m0(m, kc, r):
            i = (m * 32 + kc) * 2 + r
            return modflat[:, i:i + 1]

        def m1(m, kc, r):
            i = (m * 32 + kc) * 2 + r
            return mod1p[:, i:i + 1]

        gz = A.alloc([128, 16384], F32)
        gb = A.alloc([128, 24576], F32)
        gix = A.alloc([128, 32], I32)
        GZ, GB, GIX = Buf("gz"), Buf("gb"), Buf("gix")
        ch_gz, ch_gl, ch_gs, ch_gc, ch_gi = P.chan("ch_gz"), P.chan("ch_gl"), P.chan("ch_gs"), P.chan("ch_gc"), P.chan("ch_gi")
        P.op("pool", lambda e: e.memset(gz, 0.0), [], [GZ])

        def gather_rows(src_ext, r_own, W, idx_ext, ioff, part, allv, n_cc):
            nt = r_own // 128
            PART, ALLB = Buf("part"), Buf("all")
            dma("sp", gix[:, ioff:ioff + nt], idx_ext, ch_gi, [], [GIX])
            tot = NCORE * r_own * W // 128
            pflat = part.rearrange("(p a) w -> p (a w)", p=128)
            for o_ in range(0, tot, 16384):
                n_ = min(16384, tot - o_)
                dma("sp", pflat[:, o_:o_ + n_], gz[:, 0:n_], ch_gz, [GZ], [PART])
            for t_ in range(nt):
                dma("sp", gb[:, 0:W], src_ext[t_ * 128:(t_ + 1) * 128, :], ch_gl, [], [GB])
                P.dma("pool", lambda e, t_=t_: e.indirect_dma_start(out=part, out_offset=bass.IndirectOffsetOnAxis(ap=gix[:, ioff + t_:ioff + t_ + 1], axis=0),
                                                                     in_=gb[:, 0:W], in_offset=None), ch_gs, [GB, GIX, PART], [PART])
            rows = NCORE * r_own
            step = rows // n_cc
            for c_ in range(n_cc):
                r0, r1 = c_ * step, (c_ + 1) * step
                P.dma("pool", lambda e, r0=r0, r1=r1: e.collective_compute("AllReduce", ALU.add, replica_groups=[list(range(NCORE))],
                                                                           ins=[part[r0:r1, :].opt()], outs=[allv[r0:r1, :].opt()]), ch_gc, [PART], [ALLB], inc=1)

        gather_rows(x_own, TOWN, D, xidx_in, 0, x_part, x_full, 2)
        if phase_b:
            gather_rows(w_in_b_own, 512, 14336, widx_in, 16, w_in_b_part, w_in_b, 2)
            if stop is None:
                for q in range(4):
                    gather_rows(w_eg_own[q], 1024, 6144, eidx_in, 20, w_eg_part[q], w_eg[q], 2)
                    gather_rows(w_eu_own[q], 1024, 6144, eidx_in, 20, w_eu_part[q], w_eu[q], 2)
                for q in range(6):
                    gather_rows(w_ed_own[q], 1024, 4096, eidx_in, 20, w_ed_part[q], w_ed[q], 2)
        P.barrier()
        A.reset(persist_off)
        c2_sb = A.alloc([128, KC, 2], F32)
        sc = A.alloc([128, KC, 2], F32)
        bfm = A.alloc([128, 24], F32)
        modown = A.alloc([128, 24, 2], F32)
        zt = A.alloc([128, 8, 48], F32)
        midx = A.alloc([128, 1], I32)
        wblk = [A.alloc([128, KC, 512], F32) for _ in range(2)]
        C2, SC, BFM, MODOWN, ZT, MIDX = (Buf(n) for n in ("c2", "sc", "bfm", "modown", "zt", "midx"))
        WBLK = [Buf("wblk0"), Buf("wblk1")]
        MODP, MODA = Buf("mod_part"), Buf("mod_all")
        ch_a0 = P.chan("ch_a0")
        ch_w = [P.chan("ch_wblk0"), P.chan("ch_wblk1")]
        dma("sp", c2_sb, c2_in, ch_a0, [], [C2])
        dma("sp", bfm, b_ada_fm, ch_a0, [], [BFM])
        dma("sp", midx, modidx_in, ch_a0, [], [MIDX])
        act(sc, c2_sb, AF.Silu, [C2], [SC])
        P.op("pool", lambda e: e.memset(zt, 0.0), [], [ZT])
        ch_z = P.chan("ch_zero")
        dma("sp", mod_part.rearrange("(c p) n -> p c n", p=128), zt, ch_z, [ZT], [MODP])
        psA = banks[0][:, 0:48].rearrange("p (g r) -> p g r", r=2)
        for cb in range(6):
            wb = wblk[cb % 2]
            dma("sp", wb, w_ada_s[:, cb * 512:(cb + 1) * 512].rearrange("(k p) n -> p k n", p=128), ch_w[cb % 2], [], [WBLK[cb % 2]])
            for gg in range(4):
                g = cb * 4 + gg

                def fn(e, wb=wb, gg=gg, g=g):
                    for kc in range(KC):
                        ins = e.matmul(psA[:, g, :], lhsT=wb[:, kc, gg * 128:(gg + 1) * 128], rhs=sc[:, kc, :], start=(kc == 0), stop=(kc == KC - 1))
                    return ins
                P.op("pe", fn, [WBLK[cb % 2], SC], [BK[0]])
        tt("dve", modown, psA, bfm.unsqueeze(2).to_broadcast([128, 24, 2]), ALU.add, [BK[0], BFM], [MODOWN])
        ch_ms = P.chan("ch_modscatter")
        P.dma("pool", lambda e: e.indirect_dma_start(out=mod_part, out_offset=bass.IndirectOffsetOnAxis(ap=midx[:, :], axis=0),
                                                     in_=modown.rearrange("p g r -> p (g r)"), in_offset=None), ch_ms, [MODOWN, MIDX], [MODP])
        ch_cc = P.chan("ch_cc")
        P.dma("pool", lambda e: e.collective_compute("AllReduce", ALU.add, replica_groups=[list(range(NCORE))],
                                                     ins=[mod_part.opt()], outs=[mod_all.opt()]), ch_cc, [MODP], [MODA], inc=1)
        ch_ml = P.chan("ch_modload")
        dma("sp", modflat.rearrange("p (c n) -> p c n", n=48), mod_all.rearrange("(c p) n -> p c n", p=128), ch_ml, [MODA], [MOD])
        ts("dve", mod1p, modflat, 1.0, None, ALU.add, None, [MOD], [MOD])
        MODN = Buf("mod_nat")
        mnat = A.alloc([128, 128], F32)
        MNAT = Buf("mnat")
        ch_mn = P.chan("ch_modnat")
        for m in range(6):
            src_v = modflat[:, m * 64:(m + 1) * 64].rearrange("p (q r) -> p q r", r=2)[:, :, 0]
            P.op("pe", lambda e, src_v=src_v: e.transpose(banks[1][0:32, 0:128], in_=src_v, identity=ident_f), [MOD, CONST], [BK[1]])
            cp("act", mnat[0:32, :], banks[1][0:32, 0:128], [BK[1]], [MNAT])
            dma("sp", mod_nat[m].rearrange("(q a) -> q a", a=128), mnat[0:32, :], ch_mn, [MNAT], [MODN])
        if dbg:
            ch_dbg = P.chan("ch_dbg")
            DBG = Buf("dbg")
            dma("sp", dbg_t["mod"], modflat, ch_dbg, [MOD], [DBG])
        P.barrier()

        if stop == 'a0':
            P.emit()
            return nc
        A.reset(persist_off)
        Wq = A.alloc([128, KC, 1024], BF16)
        Wab = A.alloc([128, KC, 8], BF16)
        convq = A.alloc([128, 6, 3], F32)
        nexpA = A.alloc([128, 4], F32)
        dtb = A.alloc([128, 4], F32)
        tri4 = A.alloc([128, 4, 128], F32)
        nbu = A.alloc([128, 4, 128], F32)
        nbl = A.alloc([128, 4, 128], F32)
        ident4 = A.alloc([128, 4, 128], F32)
        lvlmask = A.alloc([128, 7, 128], BF16)
        xt = [A.alloc([128, D], F32) for _ in range(2)]
        xn = A.alloc([128, 2, D], BF16)
        hT = A.alloc([128, KC, GT], BF16)
        stat = [A.alloc([128, 8], F32) for _ in range(2)]
        pre = A.alloc([128, 6, GT], F32)
        qkvs = A.alloc([128, 6, GT], F32)
        rn = pre[:, 0:4, :]
        zb = A.alloc([128, 2, GT], BF16)
        sq = A.alloc([128, 4, GT], BF16)
        qkn = A.alloc([128, 6, GT], BF16)
        gbx = A.alloc([128, 2, 8], F32)
        gbe = A.alloc([128, 2, 4], F32)
        gccol = A.alloc([128, 4], F32)
        smalls = A.alloc([128, 8, 4], F32)
        tU = A.alloc([128, 4, 128], F32)
        tU2 = A.alloc([128, 4, 128], F32)
        n0f = tU2
        tL2 = A.alloc([128, 4, 128], F32)
        trig = tL2
        egr = tL2
        decT = A.alloc([128, 4, 128], F32)
        decN = A.alloc([128, 4, 128], F32)
        kbg = A.alloc([128, 4, 128], BF16)
        vb = A.alloc([128, 4, 128], BF16)
        Mt = [A.alloc([128, 4, 128], BF16) for _ in range(2)]
        Nt = [A.alloc([128, 4, 128], BF16) for _ in range(2)]
        NIt = A.alloc([128, 4, 128], BF16)
        Tt = [A.alloc([128, 4, 128], BF16) for _ in range(2)]
        recs = [A.alloc([128, 4, RW], BF16) for _ in range(2)]
        print("A1 arena bytes", A.off)

        names = ["Wq", "Wab", "cst", "xn", "hT", "junk", "pre", "qkvs", "zb", "sq", "rn", "qkn", "gbx", "gbe", "gccol", "smalls",
                 "trig", "tU", "tU2", "tL2", "decT", "decN", "egr", "n0f", "kbg", "vb", "NIt"]
        B_ = {n: Buf(n) for n in names}
        XT = [Buf("xt0"), Buf("xt1")]
        STAT = [Buf("stat0"), Buf("stat1")]
        MT = [Buf("M0"), Buf("M1")]
        NT = [Buf("N0"), Buf("N1")]
        TT = [Buf("T0"), Buf("T1")]
        RECS = [Buf("recs0"), Buf("recs1")]
        RECD = Buf("rec_d")
        ZSCR = Buf("z_scr")
        PBS = [Buf("pb0"), Buf("pb1")]

        ch_w1 = P.chan("ch_w1")
        P.dma("pool", lambda e: e.dma_start(out=Wq, in_=w_qkvz.rearrange("(k p) n -> p k n", p=128)), ch_w1, [], [B_["Wq"]])
        P.dma("pool", lambda e: e.dma_start(out=Wab, in_=w_ab.rearrange("(k p) n -> p k n", p=128)), ch_w1, [], [B_["Wab"]])
        ch_k1 = P.chan("ch_k1")
        for dst, src in ((convq, convq_in), (nexpA, alog_in), (dtb, dtb_in), (tri4, tri4_in), (nbu, nbu_in), (nbl, nbl_in), (ident4, ident4_in)):
            dma("sp", dst, src, ch_k1, [], [B_["cst"]])
        P.dma("pool", lambda e: e.dma_start(out=lvlmask, in_=lvlmask_in), ch_w1, [], [B_["cst"]])
        act(nexpA, nexpA, AF.Exp, [B_["cst"]], [B_["cst"]])
        ts("dve", nexpA, nexpA, -1.0, None, ALU.mult, None, [B_["cst"]], [B_["cst"]])

        ch_x = [P.chan("ch_x0"), P.chan("ch_x1")]
        ch_rec = P.chan("ch_rec")
        ch_zs = P.chan("ch_zs")

        def a1_group(src, r, row_len, chunk0, tok0, is_lat, first):
            for t_ in range(2):
                dma("sp", xt[t_], src[t_ * 128:(t_ + 1) * 128, :], ch_x[t_], [], [XT[t_]])
                s_ = stat[t_]
                P.op("dve", lambda e, t_=t_, s_=s_: e.reduce_sum(out=s_[:, 0:1], in_=xt[t_], axis=AX.X), [XT[t_]], [STAT[t_]])
                act(xn[:, t_, :], xt[t_], AF.Square, [XT[t_]], [B_["xn"], STAT[t_]], accum_out=s_[:, 1:2])
                ts("dve", s_[:, 2:3], s_[:, 0:1], 1.0 / D, None, ALU.mult, None, [STAT[t_]], [STAT[t_]])
                tt("dve", s_[:, 3:4], s_[:, 2:3], s_[:, 2:3], ALU.mult, [STAT[t_]], [STAT[t_]])
                stt("dve", s_[:, 4:5], s_[:, 1:2], 1.0 / D, s_[:, 3:4], ALU.mult, ALU.subtract, [STAT[t_]], [STAT[t_]])
                act(s_[:, 5:6], s_[:, 4:5], AF.Sqrt, [STAT[t_]], [STAT[t_]], bias=LN_EPS, scale=1.0)
                P.op("dve", lambda e, s_=s_: e.reciprocal(out=s_[:, 5:6], in_=s_[:, 5:6]), [STAT[t_]], [STAT[t_]])
                stt("dve", s_[:, 6:7], s_[:, 2:3], -1.0, s_[:, 5:6], ALU.mult, ALU.mult, [STAT[t_]], [STAT[t_]])
                ts("pool", xn[:, t_, :], xt[t_], s_[:, 5:6], s_[:, 6:7], ALU.mult, ALU.add, [XT[t_], STAT[t_]], [B_["xn"]])
            for kq in range(8):
                bi = kq % 2
                pv = bkbf(bi).rearrange("p (k t) -> p k t", t=GT)

                def fn(e, kq=kq, pv=pv):
                    for kk in range(4):
                        kc = kq * 4 + kk
                        for t_ in range(2):
                            ins = e.transpose(pv[:, kk, t_ * 128:(t_ + 1) * 128], in_=xn[:, t_, kc * 128:(kc + 1) * 128], identity=ident_b)
                    return ins
                P.op("pe", fn, [B_["xn"], CONST], [BK[bi]])
                for kk in range(4):
                    kc = kq * 4 + kk
                    act(hT[:, kc, :], pv[:, kk, :], AF.Identity, [BK[bi], MOD], [B_["hT"]], scale=m1(1, kc, r), bias=m0(0, kc, r))
            nrow = GT // row_len
            for blk in range(8):
                slot = blk % 2
                pp = banks[2][:, slot * GT:(slot + 1) * GT]
                PB = BK[2]

                def fn(e, blk=blk, pp=pp):
                    for kc in range(KC):
                        ins = e.matmul(pp, lhsT=Wq[:, kc, blk * 128:(blk + 1) * 128], rhs=hT[:, kc, :], start=(kc == 0), stop=(kc == KC - 1))
                    return ins
                P.op("pe", fn, [B_["Wq"], B_["hT"]], [PB])
                if blk < 6:
                    act(pre[:, blk, :], pp, AF.Identity, [PB, B_["cst"]], [B_["pre"]], scale=convq[:, blk, 1:2])
                    p3 = pp.rearrange("p (a b) -> p a b", b=row_len)
                    o3 = pre[:, blk, :].rearrange("p (a b) -> p a b", b=row_len)
                    stt("dve", o3[:, :, 1:], p3[:, :, :row_len - 1], convq[:, blk, 0:1], o3[:, :, 1:], ALU.mult, ALU.add, [PB, B_["cst"], B_["pre"]], [B_["pre"]])
                    stt("dve", o3[:, :, :row_len - 1], p3[:, :, 1:], convq[:, blk, 2:3], o3[:, :, :row_len - 1], ALU.mult, ALU.add, [PB, B_["cst"], B_["pre"]], [B_["pre"]])
                elif is_lat:
                    act(zb[:, blk - 6, :], pp, AF.Identity, [PB], [B_["zb"]])
            if is_lat:
                for h in range(2):
                    dma("sp", z_scr[h, :, tok0:tok0 + GT], zb[:, h, :], ch_zs, [B_["zb"]], [ZSCR])
            act(qkvs, pre, AF.Silu, [B_["pre"]], [B_["qkvs"]])
            tt("pool", sq, qkvs[:, 0:4, :], qkvs[:, 0:4, :], ALU.mult, [B_["qkvs"]], [B_["sq"]])
            for half in range(2):
                P.op("pe", lambda e, half=half: e.matmul(banks[5 + half][:, :], lhsT=ones_b, rhs=sq[:, half * 2:half * 2 + 2, :].rearrange("p a t -> p (a t)"), start=True, stop=True),
                     [B_["sq"], CONST], [BK[5 + half]])
                act(rn[:, half * 2:half * 2 + 2, :].rearrange("p a t -> p (a t)"), banks[5 + half][:, :], AF.Sqrt, [BK[5 + half]], [B_["pre"]], bias=1e-6, scale=1.0)
            P.op("dve", lambda e: e.reciprocal(out=rn, in_=rn), [B_["pre"]], [B_["pre"]])
            stt("dve", qkn[:, 0:2, :], qkvs[:, 0:2, :], 128.0 ** -0.5, rn[:, 0:2, :], ALU.mult, ALU.mult, [B_["qkvs"], B_["pre"]], [B_["qkn"]])
            tt("dve", qkn[:, 2:4, :], qkvs[:, 2:4, :], rn[:, 2:4, :], ALU.mult, [B_["qkvs"], B_["pre"]], [B_["qkn"]])
            cp("pool", qkn[:, 4:6, :], qkvs[:, 4:6, :], [B_["qkvs"]], [B_["qkn"]])
            psab = banks[3][:, 0:16].rearrange("p (t n) -> p t n", n=8)

            def fn(e):
                for t_ in range(2):
                    for kc in range(KC):
                        ins = e.matmul(psab[:, t_, :], lhsT=hT[:, kc, t_ * 128:(t_ + 1) * 128], rhs=Wab[:, kc, :], start=(kc == 0), stop=(kc == KC - 1))
                return ins
            P.op("pe", fn, [B_["hT"], B_["Wab"]], [BK[3]])
            tt("dve", gbe, psab[:, :, 0:4], dtb.unsqueeze(1).to_broadcast([128, 2, 4]), ALU.add, [BK[3], B_["cst"]], [B_["gbe"]])
            act(gbx[:, :, 4:8], psab[:, :, 4:8], AF.Sigmoid, [BK[3]], [B_["gbx"]])
            act(gbe, gbe, AF.Exp, [B_["gbe"]], [B_["gbe"]])
            act(gbe, gbe, AF.Ln, [B_["gbe"]], [B_["gbe"]], bias=1.0, scale=1.0)
            tt("dve", gbx[:, :, 0:4], gbe, nexpA.unsqueeze(1).to_broadcast([128, 2, 4]), ALU.mult, [B_["gbe"], B_["cst"]], [B_["gbx"]])
            if dbg and first:
                dma("sp", dbg_t["qkv"], qkvs, ch_dbg, [B_["qkvs"]], [DBG])
                dma("sp", dbg_t["qkn"], qkn, ch_dbg, [B_["qkn"]], [DBG])
                dma("sp", dbg_t["gb"], gbx, ch_dbg, [B_["gbx"]], [DBG])
            for t_ in range(2):
                chunk = chunk0 + t_
                tsl = slice(t_ * 128, (t_ + 1) * 128)
                rs = recs[chunk % 2]
                RS = RECS[chunk % 2]
                g4 = gbx[:, t_, 0:4]
                b4 = gbx[:, t_, 4:8]
                kT = qkn[:, 2:4, tsl]
                qT = qkn[:, 0:2, tsl]
                vT = qkn[:, 4:6, tsl]
                b3 = bkbf(3)
                ktm = b3[:, 64:320].rearrange("p (h t) -> p h t", t=128)
                vtm = b3[:, 320:576].rearrange("p (h t) -> p h t", t=128)
                pscol = banks[3][:, 16:20]
                Gp = banks[4][:, 0:256].rearrange("p (h t) -> p h t", t=128)
                QKp = banks[4][:, 256:512].rearrange("p (h t) -> p h t", t=128)
                b5 = banks[5][:, :].rearrange("p (b t) -> p b t", t=128)
                b6 = banks[6][:, :].rearrange("p (b t) -> p b t", t=128)
                b7 = banks[7][:, :].rearrange("p (b t) -> p b t", t=128)
                b6bf = bkbf(6)[:, 0:512].rearrange("p (b t) -> p b t", t=128)

                def fn(e, kT=kT, vT=vT, qT=qT, g4=g4):
                    for h in range(2):
                        e.transpose(ktm[:, h, :], in_=kT[:, h, :], identity=ident_b)
                        e.transpose(vtm[:, h, :], in_=vT[:, h, :], identity=ident_b)
                    for d in range(2):
                        ins = e.matmul(pscol[:, d * 2:d * 2 + 2], lhsT=tri4[:, d * 2, :], rhs=g4[:, d * 2:d * 2 + 2], start=True, stop=True)
                    return ins
                P.op("pe", fn, [B_["qkn"], CONST, B_["cst"], B_["gbx"]], [BK[3]])

                def fn(e, kT=kT, qT=qT):
                    for h in range(2):
                        e.matmul(Gp[:, h, :], lhsT=kT[:, h, :], rhs=kT[:, h, :], start=True, stop=True)
                        ins = e.matmul(QKp[:, h, :], lhsT=kT[:, h, :], rhs=qT[:, h, :], start=True, stop=True)
                    return ins
                P.op("pe", fn, [B_["qkn"]], [BK[4]])
                cp("act", gccol, pscol, [BK[3]], [B_["gccol"]])
                tt("pool", trig, tri4, g4.unsqueeze(2).to_broadcast([128, 4, 128]), ALU.mult, [B_["cst"], B_["gbx"]], [B_["tL2"]])
                P.op("pe", lambda e: e.matmul(banks[5][:, :], lhsT=ones_f, rhs=trig.rearrange("p b t -> p (b t)"), start=True, stop=True), [CONST, B_["tL2"]], [BK[5]])
                gcb = gccol.unsqueeze(2).to_broadcast([128, 4, 128])
                tt("dve", tU, b5, gcb, ALU.subtract, [BK[5], B_["gccol"]], [B_["tU"]])
                tt("pool", tU2, tU, nbu, ALU.add, [B_["tU"], B_["cst"]], [B_["tU2"]])
                tt("pool", tL2, nbl, tU, ALU.subtract, [B_["tU"], B_["cst"]], [B_["tL2"]])
                act(decT, tU2, AF.Exp, [B_["tU2"]], [B_["decT"]])
                act(decN, tL2, AF.Exp, [B_["tL2"]], [B_["decN"]])
                act(egr, b5, AF.Exp, [BK[5]], [B_["tL2"]])
                SM = B_["smalls"]
                egc, kbs, gsum, kds, gl, nbeta, tmp4 = (smalls[:, i, :] for i in range(7))
                act(egc, gccol, AF.Exp, [B_["gccol"]], [SM])
                tt("dve", kbs, egc, b4, ALU.mult, [SM, B_["gbx"]], [SM])
                cp("dve", gsum[:, 0:2], b5[:, 0:2, 127], [BK[5]], [SM])
                cp("dve", gsum[:, 2:4], b5[:, 2:4, 0], [BK[5]], [SM])
                tt("dve", tmp4, gsum, gccol, ALU.subtract, [SM, B_["gccol"]], [SM])
                act(kds, tmp4, AF.Exp, [SM], [SM])
                act(gl, gsum, AF.Exp, [SM], [SM])
                ts("dve", nbeta, b4, -1.0, None, ALU.mult, None, [B_["gbx"]], [SM])
                for d in range(2):
                    bs = slice(d * 2, d * 2 + 2)
                    tt("dve", kbg[:, bs, :], ktm, kbs[:, bs].unsqueeze(2).to_broadcast([128, 2, 128]), ALU.mult, [BK[3], SM], [B_["kbg"]])
                    tt("dve", rs[:, bs, 512:640], ktm, kds[:, bs].unsqueeze(2).to_broadcast([128, 2, 128]), ALU.mult, [BK[3], SM], [RS])
                    tt("dve", vb[:, bs, :], vtm, b4[:, bs].unsqueeze(2).to_broadcast([128, 2, 128]), ALU.mult, [BK[3], B_["gbx"]], [B_["vb"]])
                    tt("dve", n0f[:, bs, :], Gp, decN[:, bs, :], ALU.mult, [BK[4], B_["decN"]], [B_["tU2"]])
                    tt("dve", rs[:, bs, 384:512], QKp, decT[:, bs, :], ALU.mult, [BK[4], B_["decT"]], [RS])
                    tt("pool", rs[:, bs, 256:384], qT, egr[:, bs, :], ALU.mult, [B_["qkn"], B_["tL2"]], [RS])
                tt("dve", Nt[0], n0f, nbeta.unsqueeze(2).to_broadcast([128, 4, 128]), ALU.mult, [B_["tU2"], SM], [NT[0]])

                def fn(e):
                    for b in range(4):
                        ins = e.transpose(b6bf[:, b, :], in_=Nt[0][:, b, :], identity=ident_b)
                    return ins
                P.op("pe", fn, [NT[0], CONST], [BK[6]])
                cp("act", Mt[0], b6bf, [BK[6]], [MT[0]])
                mk0 = lvlmask[:, 0, :].unsqueeze(1).to_broadcast([128, 4, 128])
                tt("pool", Nt[1], Nt[0], mk0, ALU.mult, [NT[0], B_["cst"]], [NT[1]])
                tt("pool", Nt[1], Nt[1], ident4, ALU.add, [NT[1], B_["cst"]], [NT[1]])
                tt("pool", Tt[0], Mt[0], mk0, ALU.mult, [MT[0], B_["cst"]], [TT[0]])
                tt("pool", Tt[0], Tt[0], ident4, ALU.add, [TT[0], B_["cst"]], [TT[0]])
                Tc = [Nt[1], Nt[0]]
                TC = [NT[1], NT[0]]
                for k in range(6):
                    ci, ni = k % 2, (k + 1) % 2
                    mk = lvlmask[:, k + 1, :].unsqueeze(1).to_broadcast([128, 4, 128])
                    tt("pool", Mt[1], Mt[0], mk, ALU.mult, [MT[0], B_["cst"]], [MT[1]])

                    def fn(e, ci=ci):
                        for b in range(4):
                            ins = e.matmul(b6[:, b, :], lhsT=Mt[1][:, b, :], rhs=Tc[ci][:, b, :], start=True, stop=True)
                        return ins
                    P.op("pe", fn, [MT[1], TC[ci]], [BK[6]])
                    tt("dve", NIt, b6, ident4, ALU.add, [BK[6], B_["cst"]], [B_["NIt"]])
                    if k < 5:
                        def fn(e, ci=ci):
                            for b in range(4):
                                ins = e.matmul(b7[:, b, :], lhsT=Tt[ci][:, b, :], rhs=NIt[:, b, :], start=True, stop=True)
                            return ins
                        P.op("pe", fn, [B_["NIt"], TT[ci]], [BK[7]])
                        cp("act", Tc[ni], b7, [BK[7]], [TC[ni]])

                    def fn(e, ci=ci):
                        for b in range(4):
                            ins = e.matmul(b5[:, b, :], lhsT=NIt[:, b, :], rhs=Tt[ci][:, b, :], start=True, stop=True)
                        return ins
                    P.op("pe", fn, [B_["NIt"], TT[ci]], [BK[5]])
                    cp("act", Tt[ni], b5, [BK[5]], [TT[ni]])
                Tf, TF = Tt[0], TT[0]

                def fn(e):
                    for b in range(4):
                        e.matmul(b5[:, b, :], lhsT=Tf[:, b, :], rhs=vb[:, b, :], start=True, stop=True)
                        ins = e.matmul(b6[:, b, :], lhsT=kbg[:, b, :], rhs=Tf[:, b, :], start=True, stop=True)
                    return ins
                P.op("pe", fn, [TF, B_["vb"], B_["kbg"]], [BK[5], BK[6]])
                cp("act", rs[:, :, 128:256], b5, [BK[5]], [RS])
                ts("dve", rs[:, :, 0:128], b6, -1.0, None, ALU.mult, None, [BK[6]], [RS])
                for b in range(4):
                    cp("pool", rs[:, b, 640:642].bitcast(F32), gl[:, b:b + 1], [SM], [RS])
                dma("sp", rec_d[chunk * 4:chunk * 4 + 4].rearrange("r p w -> p r w"), rs, ch_rec, [RS], [RECD])
                if dbg and first and t_ == 0:
                    dma("sp", dbg_t["rec"].rearrange("r p w -> p r w"), rs, ch_dbg, [RS], [DBG])

        a1_group(ctx_in, 1, NCTX, 0, 0, False, False)
        for gi in range(n_lat_groups):
            a1_group(x_full[gi * GT:(gi + 1) * GT, :], 0, 64, 2 + gi * 2, gi * GT, True, gi == 0)
        P.barrier()

        if stop == 'a1':
            P.emit()
            return nc
        A.reset(persist_off)
        o_acc = A.alloc([128, 2, n_lat_tok], F32)
        S = A.alloc([128, 4, 128], F32)
        Sb = A.alloc([128, 4, 128], BF16)
        vnew = A.alloc([128, 4, 128], BF16)
        NSLOT = 3
        rb = [[A.alloc([128, RW], BF16) for _ in range(NSLOT)] for _ in range(4)]
        RB = [[Buf("rb%d_%d" % (b, s)) for s in range(NSLOT)] for b in range(4)]
        ch_rb = [[P.chan("ch_rb%d_%d" % (b, s)) for s in range(NSLOT)] for b in range(4)]
        SB_ = [Buf("S%d" % b) for b in range(4)]
        SBB = [Buf("Sb%d" % b) for b in range(4)]
        VN = [Buf("vn%d" % b) for b in range(4)]
        OACC = {}
        print("A2 arena bytes", A.off)
        P.op("dve", lambda e: e.memset(S, 0.0), [], SB_)
        P.op("pool", lambda e: e.memset(Sb, 0.0), [], SBB)
        n_lat_chunks = n_lat_groups * 2
        seq = {0: [0, 1] + [2 + i for i in range(n_lat_chunks)],
               1: [1, 0] + [2 + i for i in reversed(range(n_lat_chunks))]}
        nsteps = 2 + n_lat_chunks
        touched = set()

        def load_rec(step, b):
            d = b // 2
            chunk = seq[d][step]
            s_ = step % NSLOT
            eng = "sp" if b % 2 == 0 else "pool"
            dma(eng, rb[b][s_], rec_d[chunk * 4 + b], ch_rb[b][s_], [RECD], [RB[b][s_]])
        for b in range(4):
            load_rec(0, b)
            load_rec(1, b)
        for step in range(nsteps):
            for b in range(4):
                if step + 2 < nsteps:
                    load_rec(step + 2, b)
            for b in range(4):
                d, h = b // 2, b % 2
                chunk = seq[d][step]
                s_ = step % NSLOT
                r_ = rb[b][s_]
                R_ = RB[b][s_]
                psV = banks[b][:, 0:128]
                psO = banks[b][:, 128:256]
                psS = banks[b][:, 256:384]
                BV = BO = BS = BK[b]
                P.op("pe", lambda e, r_=r_, b=b, psV=psV: e.matmul(psV, lhsT=r_[:, 0:128], rhs=Sb[:, b, :], start=True, stop=True), [R_, SBB[b]], [BV])
                tt("dve", vnew[:, b, :], psV, r_[:, 128:256], ALU.add, [BV, R_], [VN[b]])
                if chunk >= 2:
                    def fn(e, r_=r_, b=b, psO=psO):
                        e.matmul(psO, lhsT=Sb[:, b, :], rhs=r_[:, 256:384], start=True, stop=False)
                        return e.matmul(psO, lhsT=vnew[:, b, :], rhs=r_[:, 384:512], start=False, stop=True)
                    P.op("pe", fn, [R_, SBB[b], VN[b]], [BO])
                    lc = chunk - 2
                    key = (h, lc)
                    if key not in OACC:
                        OACC[key] = Buf("oacc%d_%d" % key)
                    osl = o_acc[:, h, lc * 128:(lc + 1) * 128]
                    if key not in touched:
                        touched.add(key)
                        cp("act", osl, psO, [BO], [OACC[key]])
                    else:
                        tt("dve", osl, psO, osl, ALU.add, [BO, OACC[key]], [OACC[key]])
                P.op("pe", lambda e, r_=r_, b=b, psS=psS: e.matmul(psS, lhsT=r_[:, 512:640], rhs=vnew[:, b, :], start=True, stop=True), [R_, VN[b]], [BS])
                stt("dve", S[:, b, :], S[:, b, :], r_[:, 640:642].bitcast(F32), psS, ALU.mult, ALU.add, [SB_[b], R_, BS], [SB_[b]])
                cp("act", Sb[:, b, :], S[:, b, :], [SB_[b]], [SBB[b]])
            if dbg and step == 1:
                dma("sp", dbg_t["Sc"], S, ch_dbg, SB_, [DBG])
        if dbg:
            dma("sp", dbg_t["S"], S, ch_dbg, SB_, [DBG])
            dma("sp", dbg_t["o"], o_acc[:, :, 0:1024], ch_dbg, list(OACC.values()), [DBG])
        P.barrier()

        if stop == 'a2':
            P.emit()
            return nc
        a3_off = A.off
        OALL = Buf("o_all")
        GN = 512
        zt3 = A.alloc([128, 8192], BF16)
        ystage = [A.alloc([128, TOWN], BF16) for _ in range(2)]
        zin = [A.alloc([128, GN], BF16) for _ in range(2)]
        osq = A.alloc([128, GN], BF16)
        rs3 = A.alloc([128, GN], F32)
        sz = A.alloc([128, GN], F32)
        t3 = A.alloc([128, GN], F32)
        normw = A.alloc([128, 1], F32)
        yidx = A.alloc([128, 16], I32)
        print("A3 arena bytes", A.off)
        ZT3, OSQ, RS3, SZ, T3, K3 = (Buf(n) for n in ("zt3", "osq", "rs3", "sz", "t3", "k3"))
        YST = [Buf("yst0"), Buf("yst1")]
        ZIN = [Buf("zin0"), Buf("zin1")]
        YB = Buf("ybuf")
        YOWN = Buf("yown")
        ch_3 = P.chan("ch_a3")
        ch_zin = [P.chan("ch_zin0"), P.chan("ch_zin1")]
        ch_zf = P.chan("ch_zfill")
        ch_ys = P.chan("ch_yscatter")
        dma("sp", normw, normw_in, ch_3, [], [K3])
        dma("sp", yidx, yidx_in, ch_3, [], [K3])
        P.op("pool", lambda e: e.memset(zt3, 0.0), [], [ZT3])
        ybflat = ybuf.rearrange("(p a) t -> p (a t)", p=128)
        for k in range(32):
            dma("sp", ybflat[:, k * 8192:(k + 1) * 8192], zt3, ch_zf, [ZT3], [YB])
        n_g3 = n_lat_tok // GN
        it = 0
        for rdst in range(NCORE):
            for h in range(2):
                ys = ystage[(rdst * 2 + h) % 2]
                YS = YST[(rdst * 2 + h) % 2]
                if rdst * 4 >= n_g3:
                    continue
                for gq in range(4):
                    g3 = rdst * 4 + gq
                    if g3 >= n_g3:
                        P.op("pool", lambda e, ys=ys, gq=gq: e.memset(ys[:, gq * GN:(gq + 1) * GN], 0.0), [], [YS])
                        continue
                    zi, ZI = zin[it % 2], ZIN[it % 2]
                    ch_ = ch_zin[it % 2]
                    it += 1
                    osl = o_acc[:, h, g3 * GN:(g3 + 1) * GN]
                    dma("sp", zi, z_scr[h, :, g3 * GN:(g3 + 1) * GN], ch_, [ZSCR], [ZI])
                    act(osq, osl, AF.Square, [OALL], [OSQ])
                    P.op("pe", lambda e: e.matmul(banks[0][:, :], lhsT=ones_b, rhs=osq, start=True, stop=True), [OSQ, CONST], [BK[0]])
                    act(rs3, banks[0][:, :], AF.Sqrt, [BK[0]], [RS3], bias=1e-6, scale=1.0 / 128)
                    P.op("dve", lambda e: e.reciprocal(out=rs3, in_=rs3), [RS3], [RS3])
                    act(sz, zi, AF.Silu, [ZI], [SZ])
                    stt("dve", t3, osl, normw[:, 0:1], rs3, ALU.mult, ALU.mult, [OALL, K3, RS3], [T3])
                    tt("pool", ys[:, gq * GN:(gq + 1) * GN], t3, sz, ALU.mult, [T3, SZ], [YS])
                col = rdst * 2 + h
                P.dma("pool", lambda e, ys=ys, col=col: e.indirect_dma_start(out=ybuf, out_offset=bass.IndirectOffsetOnAxis(ap=yidx[:, col:col + 1], axis=0),
                                                                             in_=ys, in_offset=None), ch_ys, [YS, K3, YB], [YB])
        ch_rs = P.chan("ch_rs")
        P.dma("pool", lambda e: e.collective_compute("ReduceScatter", ALU.add, replica_groups=[list(range(NCORE))],
                                                     ins=[ybuf.opt()], outs=[yown.opt()]), ch_rs, [YB], [YOWN], inc=1)
        P.barrier()
        if dbg:
            A.reset(persist_off)
            ytmp = A.alloc([128, 16, TOWN], BF16)
            YT = Buf("ytmp")
            dma("sp", ytmp, yown.rearrange("(a p) t -> p a t", p=128), ch_dbg, [YOWN], [YT])
            dma("sp", dbg_t["yown"].rearrange("(a p) t -> p a t", p=128), ytmp, ch_dbg, [YT], [DBG])
            P.barrier()

        if phase_b:
            A.reset(persist_off)
            NT16 = TOWN // 128
            NBLK = 96
            convb = A.alloc([128, 16, 3], F32)
            brt = A.alloc([128, 72], F32)
            wr = A.alloc([128, KC, 72], F32)
            iota_p = A.alloc([128, 1], F32)
            ustrict = A.alloc([128, 128], F32)
            s4p1 = A.alloc([128, KC], F32)
            s3p = A.alloc([128, KC], F32)
            w12 = A.alloc([128, NT16, 2], F32)
            destf = A.alloc([128, NT16, 2], F32)
            desti = A.alloc([128, NT16, 2], I32)
            idxe = A.alloc([128, NBLK], I32)
            stb = [A.alloc([128, 8], F32) for _ in range(2)]
            pb2_off = A.off
            rsm = A.alloc([128, 256], F32)
            AA = A.alloc([128, NT16, 128], F32)
            KB_, AAB, W12B, DESTB, IDXB, RSM = (Buf(n) for n in ("kb", "aa", "w12", "dest", "idx", "rsm"))
            STB = [Buf("stb0"), Buf("stb1")]
            pb_off = A.off
            ch_kb = P.chan("ch_kb")
            for dst, src in ((convb, convb_in), (brt, brouter_in), (iota_p, iotap_in), (ustrict, ustrict_in)):
                dma("sp", dst, src, ch_kb, [], [KB_])
            dma("sp", wr, w_router.rearrange("(k p) n -> p k n", p=128), ch_kb, [], [KB_])
            dma("sp", s4p1, mod_nat[4].rearrange("(p k) -> p k", k=KC), ch_kb, [MODN], [KB_])
            dma("sp", s3p, mod_nat[3].rearrange("(p k) -> p k", k=KC), ch_kb, [MODN], [KB_])
            ts("dve", s4p1, s4p1, 1.0, None, ALU.add, None, [KB_], [KB_])

            def ln_stats(xap, XB, s_, SB, junk, JB):
                P.op("dve", lambda e: e.reduce_sum(out=s_[:, 0:1], in_=xap, axis=AX.X), [XB], [SB])
                act(junk, xap, AF.Square, [XB], [JB, SB], accum_out=s_[:, 1:2])
                ts("dve", s_[:, 2:3], s_[:, 0:1], 1.0 / D, None, ALU.mult, None, [SB], [SB])
                tt("dve", s_[:, 3:4], s_[:, 2:3], s_[:, 2:3], ALU.mult, [SB], [SB])
                stt("dve", s_[:, 4:5], s_[:, 1:2], 1.0 / D, s_[:, 3:4], ALU.mult, ALU.subtract, [SB], [SB])
                act(s_[:, 5:6], s_[:, 4:5], AF.Sqrt, [SB], [SB], bias=LN_EPS, scale=1.0)
                P.op("dve", lambda e: e.reciprocal(out=s_[:, 5:6], in_=s_[:, 5:6]), [SB], [SB])
                stt("dve", s_[:, 6:7], s_[:, 2:3], -1.0, s_[:, 5:6], ALU.mult, ALU.mult, [SB], [SB])

            X1S, N2S = Buf("x1_scr"), Buf("n2_scr")
            SGT = 512
            for sg in range(TOWN // SGT):
                tok0 = sg * SGT
                A.reset(pb_off)
                hTb = A.alloc([128, KC, SGT], BF16)
                ycT = A.alloc([128, 16, SGT], BF16)
                ygT = A.alloc([128, 16, SGT], BF16)
                mT = A.alloc([128, KC, SGT], BF16)
                r1_off = A.off
                HT, YC, YG, MT_ = Buf("hTb"), Buf("ycT"), Buf("ygT"), Buf("mT")
                xtb = [A.alloc([128, D], F32) for _ in range(2)]
                xnb = [A.alloc([128, D], BF16) for _ in range(2)]
                XTB = [Buf("xtb0"), Buf("xtb1")]
                XNB = [Buf("xnb0"), Buf("xnb1")]
                ch_xb = [P.chan(), P.chan()]
                for t_ in range(4):
                    xt_, XT_, xn_, XN_ = xtb[t_ % 2], XTB[t_ % 2], xnb[t_ % 2], XNB[t_ % 2]
                    s_, SB = stb[t_ % 2], STB[t_ % 2]
                    dma("sp", xt_, x_own[tok0 + t_ * 128:tok0 + (t_ + 1) * 128, :], ch_xb[t_ % 2], [], [XT_])
                    ln_stats(xt_, XT_, s_, SB, xn_, XN_)
                    ts("pool", xn_, xt_, s_[:, 5:6], s_[:, 6:7], ALU.mult, ALU.add, [XT_, SB], [XN_])
                    for kq in range(4):
                        bi = kq % 2
                        pv = bkbf(bi).rearrange("p (k t) -> p k t", t=128)

                        def fn(e, kq=kq, pv=pv, xn_=xn_):
                            for kk in range(8):
                                kc = kq * 8 + kk
                                ins = e.transpose(pv[:, kk, :], in_=xn_[:, kc * 128:(kc + 1) * 128], identity=ident_b)
                            return ins
                        P.op("pe", fn, [XN_, CONST], [BK[bi]])
                        for kk in range(8):
                            kc = kq * 8 + kk
                            act(hTb[:, kc, t_ * 128:(t_ + 1) * 128], pv[:, kk, :], AF.Identity, [BK[bi], MOD], [HT], scale=m1(1, kc, 0), bias=m0(0, kc, 0))
                P.barrier()
                A.reset(r1_off)
                wcb = [A.alloc([128, 3, KC, 128], BF16) for _ in range(2)]
                cg_sb = A.alloc([128, SGT], F32)
                tcb = A.alloc([128, SGT], F32)
                ucv = A.alloc([128, SGT], F32)
                WCB = [Buf("wcb0"), Buf("wcb1")]
                CGS, TCB, UCV = Buf("cgs"), Buf("tcb"), Buf("ucv")
                ch_wc = [P.chan(), P.chan()]
                for j in range(16):
                    wc, WC = wcb[j % 2], WCB[j % 2]
                    for i3 in range(3):
                        c0 = i3 * 2048 + j * 128
                        P.dma("pool", lambda e, wc=wc, i3=i3, c0=c0: e.dma_start(out=wc[:, i3, :, :], in_=w_in_b[:, c0:c0 + 128].rearrange("(k p) n -> p k n", p=128)),
                              ch_wc[j % 2], [], [WC])
                    for i3 in range(3):
                        def fn(e, wc=wc, i3=i3):
                            for kc in range(KC):
                                ins = e.matmul(banks[2 + i3][:, :], lhsT=wc[:, i3, kc, :], rhs=hTb[:, kc, :], start=(kc == 0), stop=(kc == KC - 1))
                            return ins
                        P.op("pe", fn, [WC, HT], [BK[2 + i3]])
                    cp("act", cg_sb, banks[4][:, :], [BK[4]], [CGS])
                    tt("dve", tcb, banks[2][:, :], cg_sb, ALU.mult, [BK[2], CGS], [TCB])
                    act(ucv, tcb, AF.Identity, [TCB, KB_], [UCV], scale=convb[:, j, 1:2])
                    t3_ = tcb.rearrange("p (a b) -> p a b", b=64)
                    u3_ = ucv.rearrange("p (a b) -> p a b", b=64)
                    stt("dve", u3_[:, :, 1:], t3_[:, :, :63], convb[:, j, 0:1], u3_[:, :, 1:], ALU.mult, ALU.add, [TCB, KB_, UCV], [UCV])
                    stt("dve", u3_[:, :, :63], t3_[:, :, 1:], convb[:, j, 2:3], u3_[:, :, :63], ALU.mult, ALU.add, [TCB, KB_, UCV], [UCV])
                    tt("dve", ycT[:, j, :], banks[3][:, :], ucv, ALU.mult, [BK[3], UCV], [YC])
                P.barrier()
                A.reset(r1_off)
                wgb = [A.alloc([128, 2, KC, 128], BF16) for _ in range(2)]
                wbrb = [A.alloc([128, 2, 16, 128], BF16) for _ in range(2)]
                sga = A.alloc([128, SGT], F32)
                sgb = A.alloc([128, SGT], F32)
                t1b = A.alloc([128, SGT], F32)
                t2b = A.alloc([128, SGT], F32)
                WGB = [Buf("wgb0"), Buf("wgb1")]
                WBRB = [Buf("wbr0"), Buf("wbr1")]
                SGA, SGB, T1B, T2B = Buf("sga"), Buf("sgb"), Buf("t1b"), Buf("t2b")
                ch_wg = [P.chan(), P.chan()]
                ch_yg = P.chan()
                dma("sp", ygT, yown[:, tok0:tok0 + SGT].rearrange("(j p) t -> p j t", p=128), ch_yg, [YOWN], [YG])
                for f in range(KC):
                    wg, WG, wbr, WBR = wgb[f % 2], WGB[f % 2], wbrb[f % 2], WBRB[f % 2]
                    for i2 in range(2):
                        c0 = 6144 + i2 * 4096 + f * 128
                        P.dma("pool", lambda e, wg=wg, i2=i2, c0=c0: e.dma_start(out=wg[:, i2, :, :], in_=w_in_b[:, c0:c0 + 128].rearrange("(k p) n -> p k n", p=128)),
                              ch_wg[f % 2], [], [WG])
                        wsrc = w_ba if i2 == 0 else w_bb
                        P.dma("pool", lambda e, wbr=wbr, i2=i2, wsrc=wsrc, f=f: e.dma_start(out=wbr[:, i2, :, :], in_=wsrc[:, f * 128:(f + 1) * 128].rearrange("(k p) n -> p k n", p=128)),
                              ch_wg[f % 2], [], [WBR])
                    for i2 in range(2):
                        def fn(e, wg=wg, i2=i2):
                            for kc in range(KC):
                                ins = e.matmul(banks[2 + i2][:, :], lhsT=wg[:, i2, kc, :], rhs=hTb[:, kc, :], start=(kc == 0), stop=(kc == KC - 1))
                            return ins
                        P.op("pe", fn, [WG, HT], [BK[2 + i2]])
                        src_t, SRC = (ygT, YG) if i2 == 0 else (ycT, YC)

                        def fn(e, wbr=wbr, i2=i2, src_t=src_t):
                            for j in range(16):
                                ins = e.matmul(banks[4 + i2][:, :], lhsT=wbr[:, i2, j, :], rhs=src_t[:, j, :], start=(j == 0), stop=(j == 15))
                            return ins
                        P.op("pe", fn, [WBR, SRC], [BK[4 + i2]])
                    act(sga, banks[2][:, :], AF.Sigmoid, [BK[2]], [SGA])
                    act(sgb, banks[3][:, :], AF.Sigmoid, [BK[3]], [SGB])
                    tt("dve", t1b, banks[4][:, :], sga, ALU.mult, [BK[4], SGA], [T1B])
                    tt("dve", t2b, banks[5][:, :], sgb, ALU.mult, [BK[5], SGB], [T2B])
                    tt("pool", mT[:, f, :], t1b, t2b, ALU.add, [T1B, T2B], [MT_])
                P.barrier()
                A.reset(pb_off)
                rt = A.alloc([128, 4, D], F32)
                assert A.off <= r1_off - KC * SGT * 2 + 0 or True
                A.reset(r1_off)
                wob = [A.alloc([128, KC, 256], BF16) for _ in range(2)]
                bc0 = A.alloc([128, D], F32)
                tmpo = [A.alloc([128, 256], F32) for _ in range(2)]
                RT = [Buf("rt%d" % i) for i in range(4)]
                WOB = [Buf("wob0"), Buf("wob1")]
                BC0, BC1, N2B, H2T = Buf("bc0"), Buf("bc1"), Buf("n2b"), Buf("h2T")
                TMPO = [Buf("tmpo0"), Buf("tmpo1")]
                ch_wo = [P.chan(), P.chan()]
                ch_r = P.chan()
                for t_ in range(4):
                    dma("sp", rt[:, t_, :], x_own[tok0 + t_ * 128:tok0 + (t_ + 1) * 128, :], ch_r, [], [RT[t_]])
                dma("sp", bc0, mod_nat[2:3, :].partition_broadcast(128), ch_r, [MODN], [BC0])
                for cg in range(16):
                    wo, WO = wob[cg % 2], WOB[cg % 2]
                    P.dma("pool", lambda e, wo=wo, cg=cg: e.dma_start(out=wo, in_=w_o[:, cg * 256:(cg + 1) * 256].rearrange("(k p) n -> p k n", p=128)), ch_wo[cg % 2], [], [WO])
                    csl = slice(cg * 256, (cg + 1) * 256)
                    for t_ in range(4):
                        bi = 6 + t_ % 2

                        def fn(e, wo=wo, t_=t_, bi=bi):
                            for kc in range(KC):
                                ins = e.matmul(banks[bi][:, 0:256], lhsT=mT[:, kc, t_ * 128:(t_ + 1) * 128], rhs=wo[:, kc, :], start=(kc == 0), stop=(kc == KC - 1))
                            return ins
                        P.op("pe", fn, [WO, MT_], [BK[bi]])
                        tt("dve", tmpo[t_ % 2], banks[bi][:, 0:256], bc0[:, csl], ALU.mult, [BK[bi], BC0], [TMPO[t_ % 2]])
                        stt("dve", rt[:, t_, csl], rt[:, t_, csl], ALPHA, tmpo[t_ % 2], ALU.mult, ALU.add, [RT[t_], TMPO[t_ % 2]], [RT[t_]])
                P.barrier()
                A.reset(r1_off)
                bc0 = A.alloc([128, D], F32)
                bc1 = A.alloc([128, D], F32)
                n2b = A.alloc([128, D], BF16)
                h2T = A.alloc([128, KC, 128], F32)
                BC0 = Buf("bc0b")
                dma("sp", bc0, ln1g_in, ch_r, [], [BC0])
                dma("sp", bc1, ln1b_in, ch_r, [], [BC1])
                for t_ in range(4):
                    tile_i = sg * 4 + t_
                    rows = slice(tok0 + t_ * 128, tok0 + (t_ + 1) * 128)
                    x_, XR = rt[:, t_, :], RT[t_]
                    s_, SB = stb[t_ % 2], STB[t_ % 2]
                    ln_stats(x_, XR, s_, SB, n2b, N2B)
                    ts("pool", x_, x_, s_[:, 5:6], s_[:, 6:7], ALU.mult, ALU.add, [XR, SB], [XR])
                    tt("dve", x_, x_, bc0, ALU.mult, [XR, BC0], [XR])
                    tt("pool", x_, x_, bc1, ALU.add, [XR, BC1], [XR])
                    dma("sp", x1_scr[rows, :], x_, ch_r, [XR], [X1S])
                    ln_stats(x_, XR, s_, SB, n2b, N2B)
                    ts("pool", x_, x_, s_[:, 5:6], s_[:, 6:7], ALU.mult, ALU.add, [XR, SB], [XR])
                    cp("act", n2b, x_, [XR], [N2B])
                    dma("sp", n2_scr[rows, :], n2b, ch_r, [N2B], [N2S])
                    for kq in range(8):
                        bi = kq % 2
                        pvf = banks[bi][:, :].rearrange("p (k t) -> p k t", t=128)

                        def fn(e, kq=kq, pvf=pvf, x_=x_):
                            for kk in range(4):
                                kc = kq * 4 + kk
                                ins = e.transpose(pvf[:, kk, :], in_=x_[:, kc * 128:(kc + 1) * 128], identity=ident_f)
                            return ins
                        P.op("pe", fn, [XR, CONST], [BK[bi]])
                        for kk in range(4):
                            kc = kq * 4 + kk
                            act(h2T[:, kc, :], pvf[:, kk, :], AF.Identity, [BK[bi], MOD], [H2T], scale=m1(4, kc, 0), bias=m0(3, kc, 0))

                    def fn(e):
                        for kc in range(KC):
                            ins = e.matmul(banks[2][:, 0:72], lhsT=h2T[:, kc, :], rhs=wr[:, kc, :], start=(kc == 0), stop=(kc == KC - 1))
                        return ins
                    P.op("pe", fn, [H2T, KB_], [BK[2]])
                    lgt = rsm[:, 0:72]
                    gmx, ngm, se, gg, mx1, mx2, nm2, sg1 = (rsm[:, 72 + i:73 + i] for i in range(8))
                    ohg = rsm[:, 80:88]
                    eg = rsm[:, 88:96]
                    lsel = rsm[:, 96:104]
                    oh1 = rsm[:, 104:112]
                    msk = rsm[:, 112:120]
                    oh2 = rsm[:, 120:128]
                    ohg3 = rsm[:, 128:192].rearrange("p (g j) -> p g j", j=8)
                    t88 = rsm[:, 192:256].rearrange("p (g j) -> p g j", j=8)
                    R_ = [RSM]
                    tt("dve", lgt, banks[2][:, 0:72], brt, ALU.add, [BK[2], KB_], R_)
                    P.op("dve", lambda e: e.reduce_max(out=gmx, in_=lgt[:, 0:8], axis=AX.X), R_, R_)
                    ts("dve", ohg, lgt[:, 0:8], gmx, None, ALU.is_equal, None, R_, R_)
                    ts("dve", ngm, gmx, -1.0, None, ALU.mult, None, R_, R_)
                    act(eg, lgt[:, 0:8], AF.Exp, R_, R_, bias=ngm, scale=1.0, accum_out=se)
                    P.op("dve", lambda e: e.reciprocal(out=gg, in_=se), R_, R_)
                    cp("dve", ohg3, ohg.unsqueeze(2).to_broadcast([128, 8, 8]), R_, R_)
                    tt("dve", t88, lgt[:, 8:72].rearrange("p (g j) -> p g j", j=8), ohg3, ALU.mult, R_, R_)
                    P.op("dve", lambda e: e.tensor_reduce(out=lsel, in_=t88.rearrange("p g j -> p j g"), axis=AX.X, op=ALU.add), R_, R_)
                    P.op("dve", lambda e: e.reduce_max(out=mx1, in_=lsel, axis=AX.X), R_, R_)
                    ts("dve", oh1, lsel, mx1, None, ALU.is_equal, None, R_, R_)
                    stt("dve", msk, oh1, -1e30, lsel, ALU.mult, ALU.add, R_, R_)
                    P.op("dve", lambda e: e.reduce_max(out=mx2, in_=msk, axis=AX.X), R_, R_)
                    ts("dve", oh2, msk, mx2, None, ALU.is_equal, None, R_, R_)
                    ts("dve", nm2, mx2, -1.0, None, ALU.mult, None, R_, R_)
                    act(sg1, mx1, AF.Sigmoid, R_, R_, bias=nm2, scale=1.0)
                    tt("dve", w12[:, tile_i, 0:1], sg1, gg, ALU.mult, R_, [W12B])
                    tt("dve", w12[:, tile_i, 1:2], gg, w12[:, tile_i, 0:1], ALU.subtract, R_ + [W12B], [W12B])
                    a1v = AA[:, tile_i, 0:64].rearrange("p (g j) -> p g j", j=8)
                    a2v = AA[:, tile_i, 64:128].rearrange("p (g j) -> p g j", j=8)
                    tt("dve", a1v, ohg3, oh1.unsqueeze(1).to_broadcast([128, 8, 8]), ALU.mult, R_, [AAB])
                    tt("dve", a2v, ohg3, oh2.unsqueeze(1).to_broadcast([128, 8, 8]), ALU.mult, R_, [AAB])
                P.barrier()

            if stop == 'b_sg':
                if dbg:
                    dma("sp", dbg_t["x1"], x1_scr, ch_dbg, [X1S], [DBG])
                    dma("sp", dbg_t["aa"], AA, ch_dbg, [AAB], [DBG])
                    dma("sp", dbg_t["w12"], w12, ch_dbg, [W12B], [DBG])
                P.barrier()
                P.emit()
                return nc
            A.reset(pb_off)
            pre = A.alloc([128, NT16, 128], F32)
            runb = A.alloc([128, 128], F32)
            c64 = A.alloc([128, 8, 64], F32)
            base = A.alloc([128, 128], F32)
            tmpd = A.alloc([128, 128], F32)
            thr = A.alloc([128, NBLK, 64], F32)
            pe3 = A.alloc([128, NBLK, 64], F32)
            bef = A.alloc([128, NBLK], F32)
            bef2 = A.alloc([128, NBLK], F32)
            PRE, RUNB, C64, BASE, TMPD, THR, PE3, BEF = (Buf(n) for n in ("pre", "runb", "c64", "base", "tmpd", "thr", "pe3", "bef"))
            cnt3 = A.alloc([128, 64, 32], F32)
            thr2 = A.alloc([128, 64, 32], F32)
            CNT3 = Buf("cnt3")
            ch_t = P.chan()
            dma("sp", thr, thr_in, ch_t, [], [THR])
            dma("sp", thr2, thr2_in, ch_t, [], [THR])
            P.op("dve", lambda e: e.memset(runb, 0.0), [], [RUNB])
            for ti in range(NT16):
                def fn(e, ti=ti):
                    e.matmul(banks[3][:, 0:128], lhsT=ustrict, rhs=AA[:, ti, :], start=True, stop=True)
                    return e.matmul(banks[3][:, 128:256], lhsT=ones_f, rhs=AA[:, ti, :], start=True, stop=True)
                P.op("pe", fn, [KB_, CONST, AAB], [BK[3]])
                tt("dve", pre[:, ti, :], banks[3][:, 0:128], runb, ALU.add, [BK[3], RUNB], [PRE])
                tt("dve", runb, runb, banks[3][:, 128:256], ALU.add, [BK[3], RUNB], [RUNB])
            cnt, cp127, md, padded, pad_end, ctmp, pad_start = (c64[:, i, :] for i in range(7))
            CC = [C64]
            tt("dve", cnt, runb[:, 0:64], runb[:, 64:128], ALU.add, [RUNB], CC)
            cp("dve", cnt3, cnt.unsqueeze(2).to_broadcast([128, 64, 32]), CC, [CNT3])
            tt("dve", cnt3, cnt3, thr2, ALU.is_gt, [CNT3, THR], [CNT3])
            P.op("dve", lambda e: e.tensor_reduce(out=padded, in_=cnt3, axis=AX.X, op=ALU.add), [CNT3], CC)
            ts("dve", padded, padded, 128.0, None, ALU.mult, None, CC, CC)
            cp("dve", pad_end, padded, CC, CC)
            for sft in (1, 2, 4, 8, 16, 32):
                cp("dve", ctmp, pad_end, CC, CC)
                tt("dve", pad_end[:, sft:], pad_end[:, sft:], ctmp[:, :64 - sft], ALU.add, CC, CC)
            tt("dve", pad_start, pad_end, padded, ALU.subtract, CC, CC)
            cp("dve", base[:, 0:64], pad_start, CC, [BASE])
            tt("dve", base[:, 64:128], pad_start, runb[:, 0:64], ALU.add, CC + [RUNB], [BASE])
            for ti in range(NT16):
                tt("dve", tmpd, pre[:, ti, :], base, ALU.add, [PRE, BASE], [TMPD])
                tt("dve", tmpd, tmpd, AA[:, ti, :], ALU.mult, [TMPD, AAB], [TMPD])
                P.op("dve", lambda e, ti=ti: e.tensor_reduce(out=destf[:, ti, :], in_=tmpd.rearrange("p (k e) -> p k e", e=64), axis=AX.X, op=ALU.add), [TMPD], [DESTB])
            cp("dve", desti, destf, [DESTB], [DESTB])
            cp("dve", pe3, pad_end.unsqueeze(1).to_broadcast([128, NBLK, 64]), CC, [PE3])
            tt("dve", pe3, pe3, thr, ALU.is_le, [PE3, THR], [PE3])
            P.op("dve", lambda e: e.tensor_reduce(out=bef, in_=pe3, axis=AX.X, op=ALU.add), [PE3], [BEF])
            ts("dve", bef, bef, 63.0, None, ALU.min, None, [BEF], [BEF])
            ts("dve", bef, bef, 128.0, iota_p[:, 0:1], ALU.mult, ALU.add, [BEF, KB_], [BEF])
            cp("dve", idxe, bef, [BEF], [IDXB])
            if dbg:
                dma("sp", dbg_t["aa"], AA, ch_dbg, [AAB], [DBG])
                dma("sp", dbg_t["w12"], w12, ch_dbg, [W12B], [DBG])
                dma("sp", dbg_t["destf"], destf, ch_dbg, [DESTB], [DBG])
                dma("sp", dbg_t["bef"], bef, ch_dbg, [BEF], [DBG])
            if dbg:
                dma("sp", dbg_t["x1"], x1_scr, ch_dbg, [X1S], [DBG])
            P.barrier()
            if stop == 'b_dest':
                P.emit()
                return nc
            A.reset(pb2_off)
            n2t = [A.alloc([128, D], BF16) for _ in range(2)]
            N2T = [Buf("n2t0"), Buf("n2t1")]
            MOEB, MOEY = Buf("moe_buf"), Buf("moe_y")
            ch_n2 = [P.chan(), P.chan()]
            ch_sc = P.chan()
            for ti in range(NT16):
                dma("sp", n2t[ti % 2], n2_scr[ti * 128:(ti + 1) * 128, :], ch_n2[ti % 2], [N2S], [N2T[ti % 2]])
                for k2 in range(2):
                    P.dma("pool", lambda e, ti=ti, k2=k2: e.indirect_dma_start(out=moe_buf, out_offset=bass.IndirectOffsetOnAxis(ap=desti[:, ti, k2:k2 + 1], axis=0),
                                                                                in_=n2t[ti % 2], in_offset=None), ch_sc, [N2T[ti % 2], DESTB, MOEB], [MOEB])
            P.barrier()
            A.reset(pb2_off)
            wgt = A.alloc([128, KC, 768], BF16)
            wut = A.alloc([128, KC, 768], BF16)
            wdt = A.alloc([128, 6, D], BF16)
            xg = A.alloc([128, D], BF16)
            XTm = A.alloc([128, KC, 128], BF16)
            actT = A.alloc([128, 6, 128], BF16)
            sgt = A.alloc([128, 128], F32)
            ytile = A.alloc([128, 2048], F32)
            WGT, WUT, WDT, XG, XTM, ACTT, SGT_, YT_ = (Buf(n) for n in ("wgt", "wut", "wdt", "xg", "xtm", "actT", "sgt", "ytile"))
            ch_wgt, ch_wut, ch_wdt, ch_xg, ch_y = P.chan(), P.chan(), P.chan(), P.chan(), P.chan()
            print("MoE arena bytes", A.off)
            nblk_run = NBLK if moe_blocks is None else moe_blocks
            for b in range(nblk_run):
                dma("sp", xg, moe_buf[b * 128:(b + 1) * 128, :], ch_xg, [MOEB], [XG])
                for q in range(4):
                    P.dma("pool", lambda e, q=q, b=b: e.indirect_dma_start(out=wgt[:, q * 8:(q + 1) * 8, :].rearrange("p k n -> p (k n)"), out_offset=None, in_=w_eg[q],
                                                                           in_offset=bass.IndirectOffsetOnAxis(ap=idxe[:, b:b + 1], axis=0)), ch_wgt, [IDXB], [WGT])
                    P.dma("pool", lambda e, q=q, b=b: e.indirect_dma_start(out=wut[:, q * 8:(q + 1) * 8, :].rearrange("p k n -> p (k n)"), out_offset=None, in_=w_eu[q],
                                                                           in_offset=bass.IndirectOffsetOnAxis(ap=idxe[:, b:b + 1], axis=0)), ch_wut, [IDXB], [WUT])
                for q in range(6):
                    P.dma("pool", lambda e, q=q, b=b: e.indirect_dma_start(out=wdt[:, q, :], out_offset=None, in_=w_ed[q],
                                                                           in_offset=bass.IndirectOffsetOnAxis(ap=idxe[:, b:b + 1], axis=0)), ch_wdt, [IDXB], [WDT])
                for kq in range(4):
                    bi = kq % 2
                    pv = bkbf(bi).rearrange("p (k t) -> p k t", t=128)

                    def fn(e, kq=kq, pv=pv):
                        for kk in range(8):
                            kc = kq * 8 + kk
                            ins = e.transpose(pv[:, kk, :], in_=xg.rearrange("p (a k) -> p k a", k=KC)[:, kc, :], identity=ident_b)
                        return ins
                    P.op("pe", fn, [XG, CONST], [BK[bi]])
                    ksl = slice(kq * 8, (kq + 1) * 8)
                    tt("dve", XTm[:, ksl, :], pv, s4p1[:, ksl].unsqueeze(2).to_broadcast([128, 8, 128]), ALU.mult, [BK[bi], KB_], [XTM])
                    tt("pool", XTm[:, ksl, :], XTm[:, ksl, :], s3p[:, ksl].unsqueeze(2).to_broadcast([128, 8, 128]), ALU.add, [XTM, KB_], [XTM])
                for kd_ in range(6):
                    def fn(e, kd_=kd_):
                        for kc in range(KC):
                            e.matmul(banks[2][:, 0:128], lhsT=wgt[:, kc, :].rearrange("p (a k) -> p k a", k=6)[:, kd_, :], rhs=XTm[:, kc, :], start=(kc == 0), stop=(kc == KC - 1))
                        for kc in range(KC):
                            ins = e.matmul(banks[3][:, 0:128], lhsT=wut[:, kc, :].rearrange("p (a k) -> p k a", k=6)[:, kd_, :], rhs=XTm[:, kc, :], start=(kc == 0), stop=(kc == KC - 1))
                        return ins
                    P.op("pe", fn, [WGT, WUT, XTM], [BK[2], BK[3]])
                    act(sgt, banks[2][:, 0:128], AF.Silu, [BK[2]], [SGT_])
                    tt("dve", actT[:, kd_, :], banks[3][:, 0:128], sgt, ALU.mult, [BK[3], SGT_], [ACTT])
                for hf in range(2):
                    for c4 in range(4):
                        cgi = hf * 4 + c4
                        bi = 4 + cgi % 2

                        def fn(e, cgi=cgi, bi=bi):
                            for kd_ in range(6):
                                ins = e.matmul(banks[bi][:, :], lhsT=actT[:, kd_, :], rhs=wdt[:, kd_, cgi * 512:(cgi + 1) * 512], start=(kd_ == 0), stop=(kd_ == 5))
                            return ins
                        P.op("pe", fn, [ACTT, WDT], [BK[bi]])
                        cp("act" if cgi % 2 == 0 else "dve", ytile[:, c4 * 512:(c4 + 1) * 512], banks[bi][:, :], [BK[bi]], [YT_])
                    dma("sp", moe_y[b * 128:(b + 1) * 128, hf * 2048:(hf + 1) * 2048], ytile, ch_y, [YT_], [MOEY])
            P.barrier()
            A.reset(pb2_off)
            g1 = A.alloc([128, D], F32)
            g2 = A.alloc([128, D], F32)
            x1t = A.alloc([128, D], F32)
            bcm = A.alloc([128, D], F32)
            bcg = A.alloc([128, D], F32)
            bcb = A.alloc([128, D], F32)
            G1, G2, X1T, BCF = Buf("g1"), Buf("g2"), Buf("x1t"), Buf("bcf")
            OUTB = Buf("out")
            ch_f = P.chan()
            ch_g = [P.chan(), P.chan()]
            ch_o = P.chan()
            dma("sp", bcm, mod_nat[5:6, :].partition_broadcast(128), ch_f, [MODN], [BCF])
            dma("sp", bcg, ln2g_in, ch_f, [], [BCF])
            dma("sp", bcb, ln2b_in, ch_f, [], [BCF])
            for ti in range(NT16):
                rows = slice(ti * 128, (ti + 1) * 128)
                s_, SB = stb[ti % 2], STB[ti % 2]
                P.dma("pool", lambda e, ti=ti: e.indirect_dma_start(out=g1, out_offset=None, in_=moe_y, in_offset=bass.IndirectOffsetOnAxis(ap=desti[:, ti, 0:1], axis=0)),
                      ch_g[0], [MOEY, DESTB], [G1])
                P.dma("pool", lambda e, ti=ti: e.indirect_dma_start(out=g2, out_offset=None, in_=moe_y, in_offset=bass.IndirectOffsetOnAxis(ap=desti[:, ti, 1:2], axis=0)),
                      ch_g[1], [MOEY, DESTB], [G2])
                dma("sp", x1t, x1_scr[rows, :], ch_f, [X1S], [X1T])
                ts("dve", g1, g1, w12[:, ti, 0:1], None, ALU.mult, None, [G1, W12B], [G1])
                stt("dve", g1, g2, w12[:, ti, 1:2], g1, ALU.mult, ALU.add, [G1, G2, W12B], [G1])
                tt("dve", g1, g1, bcm, ALU.mult, [G1, BCF], [G1])
                stt("dve", x1t, x1t, ALPHA, g1, ALU.mult, ALU.add, [X1T, G1], [X1T])
                ln_stats(x1t, X1T, s_, SB, g2.bitcast(BF16)[:, 0:D], G2)
                ts("pool", x1t, x1t, s_[:, 5:6], s_[:, 6:7], ALU.mult, ALU.add, [X1T, SB], [X1T])
                tt("dve", x1t, x1t, bcg, ALU.mult, [X1T, BCF], [X1T])
                tt("pool", x1t, x1t, bcb, ALU.add, [X1T, BCF], [X1T])
                dma("sp", out_d[rows, :], x1t, ch_o, [X1T], [OUTB])
        P.barrier()
        print("ops", P.n_ops, "waits", P.n_waits)
        P.emit()
    return nc


def _consts():
    p = np.arange(128)[:, None]
    f = np.arange(128)[None, :]
    ident = np.eye(128, dtype=np.float32)
    tri_f = (p <= f).astype(np.float32)
    tri_b = (p >= f).astype(np.float32)
    tri4 = np.stack([tri_f, tri_f, tri_b, tri_b], 1)
    nbu_f = np.where(f >= p, 0.0, NEG).astype(np.float32)
    nbu_b = np.where(f <= p, 0.0, NEG).astype(np.float32)
    nbu = np.stack([nbu_f, nbu_f, nbu_b, nbu_b], 1)
    nbl_f = np.where(f < p, 0.0, NEG).astype(np.float32)
    nbl_b = np.where(f > p, 0.0, NEG).astype(np.float32)
    nbl = np.stack([nbl_f, nbl_f, nbl_b, nbl_b], 1)
    ident4 = np.stack([ident] * 4, 1)
    lv = []
    for k in range(7):
        b = 1 << k
        lv.append(((p // b != f // b) & (p // (2 * b) == f // (2 * b))).astype(np.float32))
    lvlmask = np.ascontiguousarray(np.stack(lv, 1))
    return dict(ident=ident, ones=np.ones((128, 128), np.float32), tri4=np.ascontiguousarray(tri4), nbu=np.ascontiguousarray(nbu),
                nbl=np.ascontiguousarray(nbl), ident4=np.ascontiguousarray(ident4), lvlmask=lvlmask)


def make_in_maps(inp, n_lat_tok=NTOK, with_b=True, with_experts=True):
    x = inp["x"][0]
    ctx = np.ascontiguousarray(inp["ctx"][0])
    c = inp["c"][0]
    c_ctx = inp["c_ctx"]
    w_ada = inp["w_ada"][0]
    b_ada = inp["b_ada"][0]
    w_in = inp["w_in"][0]
    conv_qkv = inp["conv_qkv"][0]
    a_log = inp["a_log"][0]
    dt_bias = inp["dt_bias"][0]
    cst = _consts()
    c2 = np.ascontiguousarray(np.stack([c, c_ctx], -1).reshape(KC, 128, 2).transpose(1, 0, 2))
    conv_b = inp["conv_b"][0]
    bc = lambda v: np.ascontiguousarray(np.broadcast_to(np.asarray(v, np.float32).reshape(1, -1), (128, v.size)))
    shared = {} if not with_b else dict(
        convb=np.ascontiguousarray(conv_b.reshape(3, 16, 128).transpose(2, 1, 0)),
        w_ba=np.ascontiguousarray(inp["w_branch_a"][0]), w_bb=np.ascontiguousarray(inp["w_branch_b"][0]), w_o=np.ascontiguousarray(inp["w_out"][0]),
        ln1g_bc=bc(inp["ln1_g"][0]), ln1b_bc=bc(inp["ln1_b"][0]), ln2g_bc=bc(inp["ln2_g"][0]), ln2b_bc=bc(inp["ln2_b"][0]),
        w_router=np.ascontiguousarray(np.concatenate([inp["w_router_group"][0], inp["w_router_expert"][0]], 1)),
        brouter_bc=bc(np.concatenate([inp["b_router_group"][0], inp["b_router_expert"][0]])),
        thr=np.ascontiguousarray(np.broadcast_to((128.0 * np.arange(96, dtype=np.float32))[None, :, None], (128, 96, 64))),
        iota_p=np.arange(128, dtype=np.float32).reshape(128, 1),
        thr2=np.ascontiguousarray(np.broadcast_to((128.0 * np.arange(32, dtype=np.float32))[None, None, :], (128, 64, 32))),
        ustrict=(np.arange(128)[:, None] < np.arange(128)[None, :]).astype(np.float32),
    )
    maps = []
    for core in range(NCORE):
        h0, h1 = 2 * core, 2 * core + 1
        qcols = [hh * 128 for hh in (h0, h1)]
        blocks = [0 + qcols[0], 0 + qcols[1], 2048 + qcols[0], 2048 + qcols[1], 4096 + qcols[0], 4096 + qcols[1], 6144 + qcols[0], 6144 + qcols[1]]
        w_qkvz = np.concatenate([w_in[:, b:b + 128] for b in blocks], 1)
        acols = [8192 + 32 + d * 16 + hh for d in range(2) for hh in (h0, h1)]
        bcols = [8192 + d * 16 + hh for d in range(2) for hh in (h0, h1)]
        w_ab = w_in[:, acols + bcols]
        convq = np.stack([conv_qkv[:, b:b + 128] for b in blocks[:6]], 0)
        convq = np.ascontiguousarray(convq.transpose(2, 0, 1))
        alog4 = np.array([a_log[d, hh] for d in range(2) for hh in (h0, h1)], np.float32)
        dtb4 = np.array([dt_bias[d, hh] for d in range(2) for hh in (h0, h1)], np.float32)
        yidx = np.zeros((128, 16), np.int32)
        for r in range(NCORE):
            for h in range(2):
                yidx[:, r * 2 + h] = r * 2048 + 256 * core + h * 128 + np.arange(128)
        m = dict(
            ctx=ctx, c2=c2,
            x_own=np.ascontiguousarray(x[core * TOWN:(core + 1) * TOWN]),
            xidx=np.ascontiguousarray((core * TOWN + np.arange(16)[None, :] * 128 + np.arange(128)[:, None]).astype(np.int32)),
            w_ada_s=np.ascontiguousarray(w_ada[:, core * 3072:(core + 1) * 3072]),
            b_ada_fm=np.ascontiguousarray(b_ada[core * 3072:(core + 1) * 3072].reshape(24, 128).T),
            modidx=(core * 128 + np.arange(128, dtype=np.int32)).reshape(128, 1),
            w_qkvz=np.ascontiguousarray(w_qkvz), w_ab=np.ascontiguousarray(w_ab), convq=convq,
            alog_bc=np.ascontiguousarray(np.broadcast_to(alog4, (128, 4))), dtb_bc=np.ascontiguousarray(np.broadcast_to(dtb4, (128, 4))),
            normw=np.ascontiguousarray(inp["gdn_norm_w"][0].reshape(128, 1)), yidx=yidx,
        )
        m.update(cst)
        if with_b:
            m.update(shared)
            m["w_in_b_own"] = np.ascontiguousarray(w_in[core * 512:(core + 1) * 512, 8256:])
            m["widx"] = np.ascontiguousarray((core * 512 + np.arange(4)[None, :] * 128 + np.arange(128)[:, None]).astype(np.int32))
            if with_experts:
                m["eidx"] = np.ascontiguousarray((core * 1024 + np.arange(8)[None, :] * 128 + np.arange(128)[:, None]).astype(np.int32))
                def q4(w):
                    return np.ascontiguousarray(w.reshape(8, 128, 4, 8, 768).transpose(2, 0, 1, 3, 4)).reshape(4, 1024, 6144)
                m["w_eg_own"] = q4(inp["w_exp_gate"][0][core * 8:(core + 1) * 8])
                m["w_eu_own"] = q4(inp["w_exp_up"][0][core * 8:(core + 1) * 8])
                m["w_ed_own"] = np.ascontiguousarray(inp["w_exp_down"][0][core * 8:(core + 1) * 8].reshape(8, 128, 6, 4096).transpose(2, 0, 1, 3)).reshape(6, 1024, 4096)
        maps.append(m)
    return maps


_NC_CACHE = {}


def kernel(**inputs):
    inp = {k: np.asarray(v) for k, v in inputs.items()}
    if "nc" not in _NC_CACHE:
        _NC_CACHE["nc"] = build_program()
    nc = _NC_CACHE["nc"]
    in_maps = make_in_maps(inp)
    res = run_bass_kernel_spmd(nc, in_maps, core_ids=list(range(NCORE)))
    out = np.concatenate([r["out"] for r in res.results], 0)
    return out.reshape(1, NTOK, D).astype(np.float32)
```
